# Optimizing a Trainium2 kernel written in Bass

```python
import math
import jax, jax.numpy as jnp
from jax import lax
import numpy as np

D_MODEL = 1024
BATCH = 2
SEQ = 16384
DEPTH = 1
DEC_BATCH = 2
DEC_SEQ = 8192
PAST_LEN = 128

EPS = 1e-6
NEG = -1e30
MLA_HEADS = 8
MLA_Q_RANK = 256
MLA_KV_RANK = 128
MLA_NOPE_DIM = 64
MLA_ROPE_DIM = 32
MLA_V_DIM = 64
ROPE_THETA = 10000.0
Q_BLOCK = 128
DIL_HEADS = 8
DIL_HEAD_DIM = 64
DIL_PATTERNS = ((128, 1), (512, 4), (2048, 16))
D_MIX = MLA_HEADS * MLA_V_DIM + DIL_HEADS * DIL_HEAD_DIM
IN_SIZES = (MLA_Q_RANK, MLA_KV_RANK, MLA_ROPE_DIM,
            DIL_HEADS * DIL_HEAD_DIM, DIL_HEADS * DIL_HEAD_DIM, DIL_HEADS * DIL_HEAD_DIM)
IN_COLS = sum(IN_SIZES)
N_GROUPS = 4
EXPERTS_PER_GROUP = 4
N_EXPERTS = N_GROUPS * EXPERTS_PER_GROUP
TOP_K = 2
D_EXPERT = 512
MOE_CHUNK = 1024
N_MOD = 6

kernel_name = "hymba_mla_dilated_hmoe_encoder"


def rmsnorm(x, g):
    xf = x.astype(jnp.float32)
    y = xf * lax.rsqrt(jnp.mean(xf * xf, axis=-1, keepdims=True) + EPS)
    return (y * g.astype(jnp.float32)).astype(x.dtype)


def rope_tables(s):
    inv = ROPE_THETA ** (-jnp.arange(0, MLA_ROPE_DIM, 2, dtype=jnp.float32) / MLA_ROPE_DIM)
    ang = jnp.arange(s, dtype=jnp.float32)[:, None] * inv[None, :]
    return jnp.cos(ang), jnp.sin(ang)


def apply_rope(x, cos, sin):
    xf = x.astype(jnp.float32)
    x1, x2 = jnp.split(xf, 2, axis=-1)
    return jnp.concatenate([x1 * cos - x2 * sin, x1 * sin + x2 * cos], axis=-1).astype(x.dtype)


def mla_attention(c_q, c_kv, k_pe, q_norm_g, kv_norm_g, w_uq, w_ukv):
    b, s, _ = c_q.shape
    q = (rmsnorm(c_q, q_norm_g) @ w_uq).reshape(b, s, MLA_HEADS, MLA_NOPE_DIM + MLA_ROPE_DIM)
    q_nope, q_pe = q[..., :MLA_NOPE_DIM], q[..., MLA_NOPE_DIM:]
    kv = (rmsnorm(c_kv, kv_norm_g) @ w_ukv).reshape(b, s, MLA_HEADS, MLA_NOPE_DIM + MLA_V_DIM)
    k_nope, v = kv[..., :MLA_NOPE_DIM], kv[..., MLA_NOPE_DIM:]
    cos, sin = rope_tables(s)
    q_pe = apply_rope(q_pe, cos[None, :, None, :], sin[None, :, None, :])
    k_pe = apply_rope(k_pe, cos[None], sin[None])
    scale = (MLA_NOPE_DIM + MLA_ROPE_DIM) ** -0.5
    nblk = s // Q_BLOCK
    qn_blocks = q_nope.reshape(b, nblk, Q_BLOCK, MLA_HEADS, MLA_NOPE_DIM).transpose(1, 0, 2, 3, 4)
    qp_blocks = q_pe.reshape(b, nblk, Q_BLOCK, MLA_HEADS, MLA_ROPE_DIM).transpose(1, 0, 2, 3, 4)

    def block(args):
        qn, qp = args
        sc = (jnp.einsum('bqhd,bkhd->bhqk', qn, k_nope)
              + jnp.einsum('bqhr,bkr->bhqk', qp, k_pe)).astype(jnp.float32) * scale
        p = jax.nn.softmax(sc, axis=-1).astype(v.dtype)
        return jnp.einsum('bhqk,bkhd->bqhd', p, v)

    out = lax.map(block, (qn_blocks, qp_blocks))
    return out.transpose(1, 0, 2, 3, 4).reshape(b, s, MLA_HEADS * MLA_V_DIM)


def dilated_pattern(q, k, v, slopes, window, dilation):
    b, s, h, dh = q.shape
    r = (window // 2) // dilation
    unit = dilation * r
    s_pad = -(-s // unit) * unit
    L = s_pad // dilation
    nb = L // r

    def strided(t):
        t = jnp.pad(t, ((0, 0), (0, s_pad - s), (0, 0), (0, 0)))
        return t.reshape(b, L, dilation, h, dh).transpose(0, 2, 1, 3, 4)

    def windows(t):
        tp = jnp.pad(t, ((0, 0), (0, 0), (r, r), (0, 0), (0, 0))).reshape(b, dilation, nb + 2, r, h, dh)
        return jnp.concatenate([tp[:, :, :-2], tp[:, :, 1:-1], tp[:, :, 2:]], axis=3)

    qb = strided(q).reshape(b, dilation, nb, r, h, dh)
    kw = windows(strided(k))
    vw = windows(strided(v))
    valid = (jnp.arange(s_pad) < s).reshape(L, dilation).T
    vp = jnp.pad(valid, ((0, 0), (r, r))).reshape(dilation, nb + 2, r)
    key_valid = jnp.concatenate([vp[:, :-2], vp[:, 1:-1], vp[:, 2:]], axis=2)
    rel = jnp.arange(3 * r)[None, :] - r - jnp.arange(r)[:, None]
    in_win = jnp.abs(rel) <= r
    mask = in_win[None, None] & key_valid[:, :, None, :]
    alibi = -slopes[:, None, None] * (jnp.abs(rel) * dilation).astype(jnp.float32)[None]
    sc = jnp.einsum('bcnqhe,bcnkhe->bcnhqk', qb, kw).astype(jnp.float32) * (dh ** -0.5) + alibi
    sc = jnp.where(mask[None, :, :, None], sc, NEG)
    m = jnp.max(sc, axis=-1, keepdims=True)
    e = jnp.exp(sc - m)
    den = jnp.sum(e, axis=-1, keepdims=True)
    p = (e / den).astype(v.dtype)
    out = jnp.einsum('bcnhqk,bcnkhe->bcnqhe', p, vw)
    lse = (m[..., 0] + jnp.log(den[..., 0])).transpose(0, 1, 2, 4, 3)
    out = out.reshape(b, dilation, L, h, dh).transpose(0, 2, 1, 3, 4).reshape(b, s_pad, h, dh)[:, :s]
    lse = lse.reshape(b, dilation, L, h).transpose(0, 2, 1, 3).reshape(b, s_pad, h)[:, :s]
    return out, lse


def dilated_attention(q, k, v):
    b, s, _ = q.shape
    shp = (b, s, DIL_HEADS, DIL_HEAD_DIM)
    q, k, v = q.reshape(shp), k.reshape(shp), v.reshape(shp)
    slopes = jnp.asarray(2.0 ** (-8.0 * np.arange(1, DIL_HEADS + 1) / DIL_HEADS), dtype=jnp.float32)
    res = [dilated_pattern(q, k, v, slopes, w, d) for (w, d) in DIL_PATTERNS]
    outs = jnp.stack([o for o, _ in res], axis=0)
    lses = jnp.stack([l for _, l in res], axis=0)
    wts = jax.nn.softmax(lses, axis=0).astype(outs.dtype)
    return jnp.einsum('pbsh,pbshd->bshd', wts, outs).reshape(b, s, DIL_HEADS * DIL_HEAD_DIM)


def hier_moe(h, w_router_group, w_router_expert, w_gate, w_up, w_down):
    b, s, d = h.shape
    t = h.reshape(b * s, d)
    T = b * s
    gp = jax.nn.softmax((t @ w_router_group).astype(jnp.float32), axis=-1)
    g_idx = jnp.argmax(gp, axis=-1)
    g_w = jnp.max(gp, axis=-1)
    el = (t @ w_router_expert.reshape(d, N_EXPERTS)).reshape(T, N_GROUPS, EXPERTS_PER_GROUP)
    el = jnp.take_along_axis(el, g_idx[:, None, None], axis=1)[:, 0]
    ep = jax.nn.softmax(el.astype(jnp.float32), axis=-1)
    top_v, top_i = lax.top_k(ep, TOP_K)
    top_v = top_v / jnp.sum(top_v, axis=-1, keepdims=True)
    ids = g_idx[:, None] * EXPERTS_PER_GROUP + top_i
    wts = g_w[:, None] * top_v
    combine = jnp.sum(jax.nn.one_hot(ids, N_EXPERTS, dtype=jnp.float32) * wts[..., None], axis=1).astype(t.dtype)
    chunk = math.gcd(T, MOE_CHUNK)
    n = T // chunk

    def run(args):
        tc, wc = args
        a = jnp.einsum('cd,edf->cef', tc, w_gate)
        u = jnp.einsum('cd,edf->cef', tc, w_up)
        hid = jax.nn.silu(a) * u * wc[..., None]
        return jnp.einsum('cef,efd->cd', hid, w_down)

    out = lax.map(run, (t.reshape(n, chunk, d), combine.reshape(n, chunk, N_EXPERTS)))
    return out.reshape(b, s, d)


def encoder_layer(x, c, ada_w, ada_b, norm_mix_g, w_in, q_norm_g, kv_norm_g, w_uq, w_ukv, w_out,
                  norm_moe_g, w_router_group, w_router_expert, w_gate, w_up, w_down):
    mod = (jax.nn.silu(c) @ ada_w + ada_b)[:, None, :]
    shift_a, scale_a, gate_a, shift_m, scale_m, gate_m = jnp.split(mod, N_MOD, axis=-1)
    h = rmsnorm(x, norm_mix_g) * (1 + scale_a) + shift_a
    proj = h @ w_in
    cuts = [int(v) for v in np.cumsum(IN_SIZES)[:-1]]
    c_q, c_kv, k_pe, q_b, k_b, v_b = jnp.split(proj, cuts, axis=-1)
    y_a = mla_attention(c_q, c_kv, k_pe, q_norm_g, kv_norm_g, w_uq, w_ukv)
    y_b = dilated_attention(q_b, k_b, v_b)
    x = x + gate_a * (jnp.concatenate([y_a, y_b], axis=-1) @ w_out)
    h = rmsnorm(x, norm_moe_g) * (1 + scale_m) + shift_m
    x = x + gate_m * hier_moe(h, w_router_group, w_router_expert, w_gate, w_up, w_down)
    return x


def trunk(x, c, ada_w, ada_b, norm_mix_g, w_in, q_norm_g, kv_norm_g, w_uq, w_ukv, w_out,
          norm_moe_g, w_router_group, w_router_expert, w_gate, w_up, w_down, final_norm_g):
    for l in range(DEPTH):
        x = encoder_layer(x, c, ada_w[l], ada_b[l], norm_mix_g[l], w_in[l], q_norm_g[l], kv_norm_g[l],
                          w_uq[l], w_ukv[l], w_out[l], norm_moe_g[l], w_router_group[l],
                          w_router_expert[l], w_gate[l], w_up[l], w_down[l])
    return rmsnorm(x, final_norm_g)


def setup_inputs(seed: int = 0) -> dict:
    key = jax.random.key(seed)
    ks = jax.random.split(key, 24)
    f32 = jnp.float32

    def nrm(k, shape, scale):
        return jax.random.normal(k, shape, f32) * scale

    def gain(k, shape):
        return 1.0 + 0.02 * jax.random.normal(k, shape, f32)

    L = DEPTH
    return {
        "x_prompt": nrm(ks[0], (BATCH, SEQ, D_MODEL), 1.0),
        "x_sample": nrm(ks[1], (DEC_BATCH, DEC_SEQ, D_MODEL), 1.0),
        "c_prompt": nrm(ks[2], (BATCH, D_MODEL), 1.0),
        "c_sample": nrm(ks[3], (DEC_BATCH, D_MODEL), 1.0),
        "ada_w": nrm(ks[4], (L, D_MODEL, N_MOD * D_MODEL), 0.2 * D_MODEL ** -0.5),
        "ada_b": nrm(ks[5], (L, N_MOD * D_MODEL), 0.02),
        "norm_mix_g": gain(ks[6], (L, D_MODEL)),
        "w_in": nrm(ks[7], (L, D_MODEL, IN_COLS), D_MODEL ** -0.5),
        "q_norm_g": gain(ks[8], (L, MLA_Q_RANK)),
        "kv_norm_g": gain(ks[9], (L, MLA_KV_RANK)),
        "w_uq": nrm(ks[10], (L, MLA_Q_RANK, MLA_HEADS * (MLA_NOPE_DIM + MLA_ROPE_DIM)), MLA_Q_RANK ** -0.5),
        "w_ukv": nrm(ks[11], (L, MLA_KV_RANK, MLA_HEADS * (MLA_NOPE_DIM + MLA_V_DIM)), MLA_KV_RANK ** -0.5),
        "w_out": nrm(ks[12], (L, D_MIX, D_MODEL), D_MIX ** -0.5),
        "norm_moe_g": gain(ks[13], (L, D_MODEL)),
        "w_router_group": nrm(ks[14], (L, D_MODEL, N_GROUPS), D_MODEL ** -0.5),
        "w_router_expert": nrm(ks[15], (L, D_MODEL, N_GROUPS, EXPERTS_PER_GROUP), D_MODEL ** -0.5),
        "w_gate": nrm(ks[16], (L, N_EXPERTS, D_MODEL, D_EXPERT), D_MODEL ** -0.5),
        "w_up": nrm(ks[17], (L, N_EXPERTS, D_MODEL, D_EXPERT), D_MODEL ** -0.5),
        "w_down": nrm(ks[18], (L, N_EXPERTS, D_EXPERT, D_MODEL), D_EXPERT ** -0.5),
        "final_norm_g": gain(ks[19], (D_MODEL,)),
    }


def reference(x_prompt, x_sample, c_prompt, c_sample, ada_w, ada_b, norm_mix_g, w_in, q_norm_g, kv_norm_g,
              w_uq, w_ukv, w_out, norm_moe_g, w_router_group, w_router_expert, w_gate, w_up, w_down,
              final_norm_g):
    y_prompt = trunk(x_prompt, c_prompt, ada_w, ada_b, norm_mix_g, w_in, q_norm_g, kv_norm_g, w_uq, w_ukv,
                     w_out, norm_moe_g, w_router_group, w_router_expert, w_gate, w_up, w_down, final_norm_g)
    y_sample = trunk(x_sample, c_sample, ada_w, ada_b, norm_mix_g, w_in, q_norm_g, kv_norm_g, w_uq, w_ukv,
                     w_out, norm_moe_g, w_router_group, w_router_expert, w_gate, w_up, w_down, final_norm_g)
    return (y_prompt, y_sample)
```

```python
import numpy as np
import concourse.bass as bass
import concourse.mybir as mybir
from concourse.bass_utils import run_bass_kernel_spmd

F32 = mybir.dt.float32
BF16 = mybir.dt.bfloat16
AF = mybir.ActivationFunctionType
ALU = mybir.AluOpType
AX = mybir.AxisListType

DEBUG = False
STRICT_SAME_ENGINE = True

D = 1024
SP, SS = 16384, 8192
NP_, NS_ = 4096, 2048
HALO = 1024
EPS = 1e-6
NEXP = 16
DPAT = (1, 4, 16)


class Res:
    __slots__ = ("name", "w", "r")

    def __init__(self, name=""):
        self.name = name
        self.w = {}
        self.r = {}


class Prog:
    CENG = ("pe", "act", "dve", "pool")

    def __init__(self, nc):
        self.nc = nc
        self.engs = ["pe", "act", "dve", "pool", "sp"]
        self.ops = {e: [] for e in self.engs}
        self.pending = {e: [] for e in self.engs}
        self.nslots = {"sp": 12, "pool": 8, "act": 4}
        self.dma_count = {q: 0 for q in self.nslots}
        self.dma_tok = {q: [None] * n for q, n in self.nslots.items()}

    def _collect(self, eng, reads, writes, is_dma=False):
        waits = list(self.pending[eng])
        self.pending[eng] = []
        for r in reads:
            for t in r.w.values():
                waits.append(t)
        for w in writes:
            for t in w.w.values():
                if t[0] == "c" and t[1] == eng and not STRICT_SAME_ENGINE:
                    continue
                if is_dma and t[0] == "d":
                    continue
                waits.append(t)
            for t in w.r.values():
                if t[0] == "c" and t[1] == eng and not STRICT_SAME_ENGINE:
                    continue
                waits.append(t)
        if eng == "pe":
            waits = [t for t in waits if not (t[0] == "c" and t[1] == "pe")]
        return waits

    def op(self, eng, fn, reads=(), writes=()):
        waits = self._collect(eng, reads, writes)
        idx = len(self.ops[eng])
        tok = ("c", eng, idx)
        self.ops[eng].append(dict(fn=fn, waits=waits, kind="c"))
        for r in reads:
            r.r[eng] = tok
        for w in writes:
            w.w[eng] = tok
        return tok

    def dma(self, q, out, in_, reads=(), writes=()):
        waits = self._collect(q, reads, writes, is_dma=True)
        k = self.dma_count[q]
        ns = self.nslots[q]
        slot = k % ns
        value = 16 * (k // ns + 1)
        self.dma_count[q] = k + 1
        if self.dma_tok[q][slot] is not None:
            waits.append(self.dma_tok[q][slot])
        tok = ("d", q, slot, value)
        self.dma_tok[q][slot] = tok
        self.ops[q].append(dict(fn=lambda e: e.dma_start(out=out, in_=in_), waits=waits, kind="d", q=q, slot=slot))
        key = (q, slot)
        for r in reads:
            r.r[key] = tok
        for w in writes:
            w.w[key] = tok
        return tok

    def barrier(self):
        toks = []
        for e in self.CENG:
            n = len(self.ops[e])
            for i in range(n - 1, -1, -1):
                if self.ops[e][i]["kind"] == "c":
                    toks.append(("c", e, i))
                    break
        for q in self.nslots:
            for t in self.dma_tok[q]:
                if t is not None:
                    toks.append(t)
        for e in self.engs:
            self.pending[e].extend(toks)

    def emit(self, block, csem, dsem):
        needed = {e: set() for e in self.CENG}
        for e in self.engs:
            for o in self.ops[e]:
                for t in o["waits"]:
                    if t[0] == "c":
                        needed[t[1]].add(t[2])
        final = []
        for q in self.nslots:
            for t in self.dma_tok[q]:
                if t is not None:
                    final.append(t)
        rank = {e: {idx: i + 1 for i, idx in enumerate(sorted(needed[e]))} for e in self.CENG}

        def body(ename):
            def f(eh):
                waited = {}

                def do_wait(t):
                    if t[0] == "c":
                        sem = csem[t[1]]
                        val = rank[t[1]][t[2]]
                        key = t[1]
                    else:
                        sem = dsem[t[1]][t[2]]
                        val = t[3]
                        key = (t[1], t[2])
                    if waited.get(key, 0) >= val:
                        return
                    eh.wait_ge(sem, val)
                    waited[key] = val

                for idx, o in enumerate(self.ops[ename]):
                    for t in o["waits"]:
                        do_wait(t)
                    ins = o["fn"](eh)
                    if o["kind"] == "c":
                        if ename in rank and idx in rank[ename]:
                            ins.then_inc(csem[ename], 1)
                    else:
                        ins.then_inc(dsem[o["q"]][o["slot"]], 16)
                if ename == "sp":
                    for t in final:
                        do_wait(t)
            return f

        block.tensor(body("pe"))
        block.scalar(body("act"))
        block.vector(body("dve"))
        block.gpsimd(body("pool"))
        block.sync(body("sp"))

    def mm(self, out, lhsT, rhs, start, stop, R, W):
        self.op("pe", lambda e: e.matmul(out, lhsT=lhsT, rhs=rhs, start=start, stop=stop), R, W)

    def tr(self, out, in_, ident, R, W):
        self.op("pe", lambda e: e.transpose(out, in_, ident), R, W)

    def act(self, out, in_, func, R, W, bias=None, scale=None):
        kw = {}
        if bias is not None:
            kw["bias"] = bias
        if scale is not None:
            kw["scale"] = scale
        self.op("act", lambda e: e.activation(out=out, in_=in_, func=func, **kw), R, W)

    def tt(self, eng, out, in0, in1, op, R, W):
        self.op(eng, lambda e: e.tensor_tensor(out=out, in0=in0, in1=in1, op=op), R, W)

    def ts(self, eng, out, in0, s1, op0, R, W, s2=None, op1=None):
        if op1 is None:
            self.op(eng, lambda e: e.tensor_scalar(out=out, in0=in0, scalar1=s1, scalar2=None, op0=op0), R, W)
        else:
            self.op(eng, lambda e: e.tensor_scalar(out=out, in0=in0, scalar1=s1, scalar2=s2, op0=op0, op1=op1), R, W)

    def stt(self, eng, out, in0, scalar, in1, op0, op1, R, W):
        self.op(eng, lambda e: e.scalar_tensor_tensor(out=out, in0=in0, scalar=scalar, in1=in1, op0=op0, op1=op1), R, W)

    def copy(self, eng, out, in_, R, W):
        if eng == "act":
            self.op("act", lambda e: e.copy(out=out, in_=in_), R, W)
        else:
            self.op(eng, lambda e: e.tensor_copy(out=out, in_=in_), R, W)

    def memset(self, eng, ap, val, W):
        self.op(eng, lambda e: e.memset(ap, val), (), W)

    def recip(self, out, in_, R, W):
        self.op("dve", lambda e: e.reciprocal(out=out, in_=in_), R, W)

    def reduce(self, out, in_, op, R, W):
        self.op("dve", lambda e: e.tensor_reduce(out=out, in_=in_, axis=AX.X, op=op), R, W)


class Arena:
    def __init__(self, t, nfloats):
        self.t = t
        self.n = nfloats * 4
        self.off = 0

    def mark(self):
        return self.off

    def release(self, m):
        self.off = m

    def f32(self, n, parts=128):
        o = (self.off + 31) // 32 * 32
        assert o + 4 * n <= self.n, f"arena overflow {o + 4 * n} > {self.n}"
        self.off = o + 4 * n
        return self.t[0:parts, o // 4:o // 4 + n]

    def bf(self, n, parts=128):
        nn = (n + 1) // 2
        return self.f32(nn, parts).bitcast(BF16)[:, 0:n]


def build_program():
    nc = bass.Bass("TRN2", target_bir_lowering=False)
    skind = "ExternalOutput" if DEBUG else "Internal"

    def din(name, shape, dt=F32):
        return nc.dram_tensor(name, list(shape), dt, kind="ExternalInput").ap()

    def dscr(name, shape, dt=BF16):
        return nc.dram_tensor(name, list(shape), dt, kind=skind).ap()

    xT = {"p": din("xTp", [D, SP]), "s": din("xTs", [D, SS])}
    vecs = din("vecs", [128, 96])
    ada_w = din("ada_w", [D, 6 * D])
    w_in = din("w_in", [D, 1952])
    w_pe_sw = din("w_pe_sw", [D, 32])
    w_uq = din("w_uq", [256, 768])
    w_uq_sw = din("w_uq_sw", [256, 768])
    w_ukv = din("w_ukv", [128, 1024])
    w_out = din("w_out", [D, D])
    w_r = din("w_r", [D, 20])
    w_gate = din("w_gate", [NEXP, D, 512])
    w_up = din("w_up", [NEXP, D, 512])
    w_down = din("w_down", [NEXP, 512, D])
    cs_in = {"p": din("cs_p", [32, SP]), "s": din("cs_s", [32, SS])}
    sn_in = {"p": din("sn_p", [32, SP]), "s": din("sn_s", [32, SS])}
    valid_in = {"p": din("valid_p", [128, 48]), "s": din("valid_s", [128, 32])}
    dmask_in = din("dmask", [128, 48 * 128])
    ident_in = din("ident", [128, 128])
    sel_in = din("sel", [16, NEXP * 128])

    yT = {"p": nc.dram_tensor("yTp", [D, NP_], F32, kind="ExternalOutput").ap(),
          "s": nc.dram_tensor("yTs", [D, NS_], F32, kind="ExternalOutput").ap()}

    SEQ = {"p": SP, "s": SS}
    NOWN = {"p": NP_, "s": NS_}
    SIDX = {"p": 0, "s": 1}
    ckvn_d = {s: dscr("ckvn_" + s, [128, SEQ[s]]) for s in "ps"}
    kpe_d = {s: dscr("kpe_" + s, [32, SEQ[s]]) for s in "ps"}
    qm_d = {s: dscr("qm_" + s, [8, 96, NOWN[s]]) for s in "ps"}
    qd_d = {s: dscr("qd_" + s, [512, NOWN[s]]) for s in "ps"}
    kd_d = {s: dscr("kd_" + s, [512, NOWN[s] + 2 * HALO]) for s in "ps"}
    vd_d = {s: dscr("vd_" + s, [NOWN[s] + 2 * HALO, 520]) for s in "ps"}
    ya_d = {s: dscr("ya_" + s, [512, NOWN[s]]) for s in "ps"}
    yb_d = {s: dscr("yb_" + s, [512, NOWN[s]]) for s in "ps"}
    R_ckvn = {s: Res() for s in "ps"}
    R_kpe = {s: Res() for s in "ps"}
    R_qm = {s: Res() for s in "ps"}
    R_qd = {s: Res() for s in "ps"}
    R_kd = {s: Res() for s in "ps"}
    R_vd = {s: Res() for s in "ps"}
    R_ya = {s: Res() for s in "ps"}
    R_yb = {s: Res() for s in "ps"}

    NARENA = 52000
    from contextlib import ExitStack
    with ExitStack() as es:
        arena_t = es.enter_context(nc.sbuf_tensor("arena", [128, NARENA], F32))
        tri = [es.enter_context(nc.psum_tensor(f"tri{i}", [128, 1536], F32)) for i in range(2)]
        tri = [p_[:, :] for p_ in tri]
        pair_ = es.enter_context(nc.psum_tensor("pair", [128, 1024], F32))[:, :]
        banks = [tri[i // 3][:, (i % 3) * 512:(i % 3 + 1) * 512] for i in range(6)]
        banks += [pair_[:, 0:512], pair_[:, 512:1024]]
        RB = [Res(f"bank{i}") for i in range(8)]
        P = Prog(nc)
        csem = {e: es.enter_context(nc.semaphore("c_" + e)) for e in Prog.CENG}
        dsem = {q: [es.enter_context(nc.semaphore(f"d_{q}{i}")) for i in range(n)] for q, n in P.nslots.items()}
        A = Arena(arena_t, NARENA)

        vec = A.f32(96)
        R_vec = Res()
        P.dma("sp", vec, vecs, (), (R_vec,))
        cT = vec[:, 0:16].rearrange("p (j s) -> p j s", s=2)
        ada_b = vec[:, 16:64]
        g_mix = vec[:, 64:72]
        g_moe = vec[:, 72:80]
        g_fin = vec[:, 80:88]
        qg = vec[:, 88:90]
        kvg = vec[:, 90:91]
        ones_bf = A.bf(128)
        ones32 = A.f32(64)
        R_const = Res()
        P.memset("dve", ones_bf, 1.0, (R_const,))
        P.memset("dve", ones32, 1.0, (R_const,))
        ident = A.f32(128)
        P.dma("sp", ident, ident_in, (), (R_const,))
        mod = A.f32(96).rearrange("p (m s) -> p m s", s=2)
        g1s = A.f32(16).rearrange("p (j s) -> p j s", s=2)
        g2s = A.f32(16).rearrange("p (j s) -> p j s", s=2)
        R_mod = Res()

        m0 = A.mark()
        sc = A.f32(16).rearrange("p (j s) -> p j s", s=2)
        R_sc = Res()
        P.act(sc, cT, AF.Silu, (R_vec,), (R_sc,))
        adaw_v = ada_w.rearrange("(j p) c -> p j c", p=128)
        pieces = [A.f32(8 * 768).rearrange("p (j c) -> p j c", c=768) for _ in range(2)]
        R_piece = [Res(), Res()]
        for q in range(8):
            pc = pieces[q % 2]
            P.dma("sp", pc, adaw_v[:, :, q * 768:(q + 1) * 768], (), (R_piece[q % 2],))
            for mm_ in range(6):
                m = q * 6 + mm_
                for j in range(8):
                    P.mm(banks[0][:, 2 * m:2 * m + 2], pc[:, j, mm_ * 128:(mm_ + 1) * 128], sc[:, j, :],
                         j == 0, j == 7, (R_piece[q % 2], R_sc), (RB[0],))
        P.tt("dve", mod, banks[0][:, 0:96].rearrange("p (m s) -> p m s", s=2),
             ada_b.unsqueeze(2).broadcast_to([128, 48, 2]), ALU.add, (RB[0], R_vec), (R_mod,))
        P.ts("dve", g1s, mod[:, 8:16, :], 1.0, ALU.add, (R_mod,), (R_mod,))
        P.tt("dve", g1s, g1s, g_mix.unsqueeze(2).broadcast_to([128, 8, 2]), ALU.mult, (R_mod, R_vec), (R_mod,))
        P.ts("dve", g2s, mod[:, 32:40, :], 1.0, ALU.add, (R_mod,), (R_mod,))
        P.tt("dve", g2s, g2s, g_moe.unsqueeze(2).broadcast_to([128, 8, 2]), ALU.mult, (R_mod, R_vec), (R_mod,))
        P.barrier()
        A.release(m0)

        def rstd_from_ss(out_sb, ss_ps, inv_n, R, W, tmp):
            P.act(tmp, ss_ps, AF.Ln, R, W, bias=eps_ap, scale=inv_n)
            P.act(out_sb, tmp, AF.Exp, W, W, scale=-0.5)

        eps_t = A.f32(1)
        eps_ap = eps_t[:, 0:1]
        P.memset("dve", eps_ap, EPS, (R_const,))

        m1 = A.mark()
        R_w1 = Res()
        wib = A.bf(8 * 1952).rearrange("p (j c) -> p j c", c=1952)
        wpsw = A.bf(8 * 32).rearrange("p (j c) -> p j c", c=32)
        wuq = A.bf(2 * 768).rearrange("p (j c) -> p j c", c=768)
        wuqs = A.bf(2 * 768).rearrange("p (j c) -> p j c", c=768)
        mst = A.mark()
        stg = [A.f32(1952) for _ in range(2)]
        R_stg = [Res(), Res()]
        k = 0
        for j in range(8):
            P.dma("sp", stg[k % 2], w_in[j * 128:(j + 1) * 128, :], (), (R_stg[k % 2],))
            P.copy("dve" if k % 2 == 0 else "pool", wib[:, j, :], stg[k % 2], (R_stg[k % 2],), (R_w1,))
            k += 1
        P.dma("sp", stg[k % 2][:, 0:256].rearrange("p (j c) -> p j c", c=32),
              w_pe_sw.rearrange("(j p) c -> p j c", p=128), (), (R_stg[k % 2],))
        P.copy("dve", wpsw, stg[k % 2][:, 0:256].rearrange("p (j c) -> p j c", c=32), (R_stg[k % 2],), (R_w1,))
        k += 1
        for (src, dst) in ((w_uq, wuq), (w_uq_sw, wuqs)):
            P.dma("sp", stg[k % 2][:, 0:1536].rearrange("p (j c) -> p j c", c=768),
                  src.rearrange("(j p) c -> p j c", p=128), (), (R_stg[k % 2],))
            P.copy("dve", dst, stg[k % 2][:, 0:1536].rearrange("p (j c) -> p j c", c=768), (R_stg[k % 2],), (R_w1,))
            k += 1
        C_CQ, C_KV, C_PE, C_QD, C_KD, C_VD = 0, 256, 384, 416, 928, 1440

        xt = [A.f32(8 * 512).rearrange("p (j c) -> p j c", c=512) for _ in range(3)]
        R_x = [Res(), Res(), Res()]
        sq = A.bf(8 * 512).rearrange("p (j c) -> p j c", c=512)
        R_sq = Res()
        h32 = A.f32(4 * 512).rearrange("p (j c) -> p j c", c=512)
        R_h32 = Res()
        hb = [A.bf(8 * 512).rearrange("p (j c) -> p j c", c=512) for _ in range(2)]
        R_hb = [Res(), Res()]
        rstd = A.f32(512)
        lnt = A.f32(512)
        R_rstd = Res()
        rstd2 = A.f32(512)
        lnt2 = A.f32(512)
        R_rstd2 = Res()
        sq2 = A.bf(2 * 512).rearrange("p (j c) -> p j c", c=512)
        R_sq2 = Res()
        ckvn_sb = [A.bf(512) for _ in range(2)]
        R_ckvn_sb = [Res(), Res()]
        cst = [A.f32(512, 32) for _ in range(2)]
        snt = [A.f32(512, 32) for _ in range(2)]
        R_cs = [Res(), Res()]
        t1 = A.f32(512)
        t2 = A.f32(512)
        R_t12 = Res()
        kper = [A.bf(512, 32) for _ in range(2)]
        R_kper = [Res(), Res()]
        ev = [A.bf(4 * 512).rearrange("p (j c) -> p j c", c=512) for _ in range(2)]
        R_ev = [Res() for _ in range(2)]
        vev = [A.bf(4 * 520).rearrange("p (j c) -> p j c", c=520) for _ in range(2)]
        R_vev = [Res(), Res()]
        cqn = A.bf(2 * 512).rearrange("p (j c) -> p j c", c=512)
        R_cqn = Res()
        qev = [A.bf(512) for _ in range(2)]
        R_qev = [Res(), Res()]
        csq = [A.f32(512) for _ in range(2)]
        snq = [A.f32(512) for _ in range(2)]
        R_csq = [Res(), Res()]
        valid = {s: A.f32(48) for s in "ps"}
        R_valid = Res()
        P.dma("sp", valid["p"], valid_in["p"], (), (R_valid,))
        P.dma("sp", valid["s"][:, 0:32], valid_in["s"], (), (R_valid,))
        for b in range(2):
            P.memset("pool", vev[b], 0.0, (R_vev[b],))

        cnt = {"ev": 0, "vev": 0, "qev": 0, "rot": 0}
        ROT = [5, 6, 7]

        def nextbank():
            b = ROT[cnt["rot"] % 3]
            cnt["rot"] += 1
            return b

        tiles = []
        for s in "ps":
            for t in range(SEQ[s] // 512):
                tiles.append((s, t))

        rstd_b = [rstd, A.f32(512)]
        R_h32j = [Res() for _ in range(8)]
        R_rstd_b = [R_rstd, Res()]

        def stageA1(i):
            s, t = tiles[i]
            b = i % 2
            x3 = i % 3
            c0 = t * 512
            P.dma("sp", xt[x3], xT[s].rearrange("(j p) c -> p j c", p=128)[:, :, c0:c0 + 512], (), (R_x[x3],))
            P.act(sq, xt[x3], AF.Square, (R_x[x3],), (R_sq,))
            yield
            for j in range(8):
                P.mm(banks[0], ones_bf, sq[:, j, :], j == 0, j == 7, (R_sq, R_const), (RB[0],))
            yield
            P.act(lnt, banks[0], AF.Ln, (RB[0], R_const), (R_rstd_b[b],), bias=eps_ap, scale=1.0 / D)
            yield
            P.act(rstd_b[b], lnt, AF.Exp, (R_rstd_b[b],), (R_rstd_b[b],), scale=-0.5)
            yield

        def stageB1(i):
            s, t = tiles[i]
            si = SIDX[s]
            b = i % 2
            x3 = i % 3
            for j in range(8):
                j4 = j % 4
                P.tt("dve", h32[:, j4, :], xt[x3][:, j, :], rstd_b[b], ALU.mult, (R_x[x3], R_rstd_b[b]), (R_h32j[j4],))
                if j % 2 == 0:
                    P.act(hb[b][:, j, :], h32[:, j4, :], AF.Identity, (R_h32j[j4], R_mod), (R_hb[b],),
                          bias=mod[:, j, si:si + 1], scale=g1s[:, j, si:si + 1])
                else:
                    P.ts("dve", hb[b][:, j, :], h32[:, j4, :], g1s[:, j, si:si + 1], ALU.mult, (R_h32j[j4], R_mod), (R_hb[b],),
                         s2=mod[:, j, si:si + 1], op1=ALU.add)
                yield

        def proj(bank, wcols, b, M=128, w=None):
            w = wib if w is None else w
            for j in range(8):
                P.mm(banks[bank][0:M, :], w[:, j, wcols:wcols + M], hb[b][:, j, :], j == 0, j == 7,
                     (R_hb[b], R_w1), (RB[bank],))

        def main(i):
            s, t = tiles[i]
            si = SIDX[s]
            b = i % 2
            c0 = t * 512
            n = NOWN[s]
            proj(1, C_KV, b)
            yield
            proj(2, C_PE, b, M=32)
            proj(3, 0, b, M=32, w=wpsw)
            yield
            P.dma("sp", cst[b], cs_in[s][:, c0:c0 + 512], (), (R_cs[b],))
            P.dma("sp", snt[b], sn_in[s][:, c0:c0 + 512], (), (R_cs[b],))
            P.act(sq2[:, 0, :], banks[1], AF.Square, (RB[1],), (R_sq2,))
            P.mm(banks[4], ones_bf, sq2[:, 0, :], True, True, (R_sq2, R_const), (RB[4],))
            rstd_from_ss(rstd2, banks[4], 1.0 / 128, (RB[4], R_const), (R_rstd2,), lnt2)
            yield
            P.stt("dve", ckvn_sb[b], banks[1], kvg, rstd2, ALU.mult, ALU.mult, (RB[1], R_rstd2, R_vec), (R_ckvn_sb[b],))
            P.dma("pool", ckvn_d[s][:, c0:c0 + 512], ckvn_sb[b], (R_ckvn_sb[b],), (R_ckvn[s],))
            yield
            P.tt("dve", t1[0:32, :], banks[2][0:32, :], cst[b], ALU.mult, (RB[2], R_cs[b]), (R_t12,))
            P.tt("dve", t2[0:32, :], banks[3][0:32, :], snt[b], ALU.mult, (RB[3], R_cs[b]), (R_t12,))
            P.tt("dve", kper[b], t1[0:32, :], t2[0:32, :], ALU.add, (R_t12,), (R_kper[b],))
            P.dma("pool", kpe_d[s][:, c0:c0 + 512], kper[b], (R_kper[b],), (R_kpe[s],))
            yield
            if c0 < n + 2 * HALO:
                e = cnt["ev"] % 2
                cnt["ev"] += 1
                for c in range(4):
                    bk = nextbank()
                    proj(bk, C_KD + c * 128, b)
                    P.copy("act" if c % 2 == 0 else "dve", ev[e][:, c, :], banks[bk], (RB[bk],), (R_ev[e],))
                    yield
                P.dma("pool", kd_d[s].rearrange("(c p) n -> p c n", p=128)[:, :, c0:c0 + 512], ev[e], (R_ev[e],), (R_kd[s],))
                v = cnt["vev"] % 2
                cnt["vev"] += 1
                for tc in range(4):
                    bk = nextbank()
                    for j in range(8):
                        P.mm(banks[bk], hb[b][:, j, tc * 128:(tc + 1) * 128], wib[:, j, C_VD:C_VD + 512], j == 0, j == 7,
                             (R_hb[b], R_w1), (RB[bk],))
                    vcol = valid[s][:, t * 4 + tc:t * 4 + tc + 1]
                    P.ts("dve", vev[v][:, tc, :].rearrange("p (h c) -> p h c", c=65)[:, :, 0:64],
                         banks[bk].rearrange("p (h c) -> p h c", c=64), vcol, ALU.mult, (RB[bk], R_valid), (R_vev[v],))
                    P.copy("pool", vev[v][:, tc, :].rearrange("p (h c) -> p h c", c=65)[:, :, 64:65],
                           vcol.unsqueeze(1).broadcast_to([128, 8, 1]), (R_valid,), (R_vev[v],))
                    yield
                P.dma("pool", vd_d[s][c0:c0 + 512, :].rearrange("(c p) f -> p c f", p=128), vev[v], (R_vev[v],), (R_vd[s],))
            if HALO <= c0 < HALO + n:
                q0 = c0 - HALO
                e = cnt["ev"] % 2
                cnt["ev"] += 1
                for c in range(4):
                    bk = nextbank()
                    proj(bk, C_QD + c * 128, b)
                    P.copy("act" if c % 2 == 0 else "dve", ev[e][:, c, :], banks[bk], (RB[bk],), (R_ev[e],))
                    yield
                P.dma("pool", qd_d[s].rearrange("(c p) n -> p c n", p=128)[:, :, q0:q0 + 512], ev[e], (R_ev[e],), (R_qd[s],))
                bks = [nextbank(), nextbank()]
                for c in range(2):
                    proj(bks[c], C_CQ + c * 128, b)
                    P.act(sq2[:, c, :], banks[bks[c]], AF.Square, (RB[bks[c]],), (R_sq2,))
                for c in range(2):
                    P.mm(banks[4], ones_bf, sq2[:, c, :], c == 0, c == 1, (R_sq2, R_const), (RB[4],))
                rstd_from_ss(rstd2, banks[4], 1.0 / 256, (RB[4], R_const), (R_rstd2,), lnt2)
                yield
                for c in range(2):
                    P.stt("dve", cqn[:, c, :], banks[bks[c]], qg[:, c:c + 1], rstd2, ALU.mult, ALU.mult,
                          (RB[bks[c]], R_rstd2, R_vec), (R_cqn,))
                P.dma("sp", csq[b][64:96, :], cs_in[s][:, c0:c0 + 512], (), (R_csq[b],))
                P.dma("sp", snq[b][64:96, :], sn_in[s][:, c0:c0 + 512], (), (R_csq[b],))
                for h in range(8):
                    ba = nextbank()
                    bb = nextbank()
                    for c in range(2):
                        P.mm(banks[ba][0:96, :], wuq[:, c, h * 96:(h + 1) * 96], cqn[:, c, :], c == 0, c == 1,
                             (R_cqn, R_w1), (RB[ba],))
                    for c in range(2):
                        P.mm(banks[bb][0:96, :], wuqs[:, c, h * 96:(h + 1) * 96], cqn[:, c, :], c == 0, c == 1,
                             (R_cqn, R_w1), (RB[bb],))
                    qe = cnt["qev"] % 2
                    cnt["qev"] += 1
                    P.copy("act", qev[qe][0:64, :], banks[ba][0:64, :], (RB[ba],), (R_qev[qe],))
                    P.tt("dve", t1[64:96, :], banks[ba][64:96, :], csq[b][64:96, :], ALU.mult, (RB[ba], R_csq[b]), (R_t12,))
                    P.tt("dve", t2[64:96, :], banks[bb][64:96, :], snq[b][64:96, :], ALU.mult, (RB[bb], R_csq[b]), (R_t12,))
                    P.tt("dve", qev[qe][64:96, :], t1[64:96, :], t2[64:96, :], ALU.add, (R_t12,), (R_qev[qe],))
                    P.dma("pool", qm_d[s][h, :, q0:q0 + 512], qev[qe][0:96, :], (R_qev[qe],), (R_qm[s],))
                    yield

        NT = len(tiles)

        def _empty():
            return
            yield

        for i in range(NT + 2):
            gens = [main(i - 2) if 2 <= i else _empty(),
                    stageB1(i - 1) if 1 <= i <= NT else _empty(),
                    stageA1(i) if i < NT else _empty()]
            done = [False, False, False]
            while not all(done):
                for gi in range(3):
                    if not done[gi]:
                        try:
                            next(gens[gi])
                        except StopIteration:
                            done[gi] = True
        P.barrier()
        A.release(m1)

        def finalize(o_sb, ncols, out_bf, bank, R_o, R_out, rden, R_rden):
            P.recip(rden[64:65, 0:ncols], o_sb[64:65, 0:ncols], (R_o,), (R_rden,))
            P.mm(banks[bank][0:64, 0:ncols], ones32[64:65, 0:64], rden[64:65, 0:ncols], True, True,
                 (R_rden, R_const), (RB[bank],))
            P.tt("dve", out_bf, o_sb[0:64, 0:ncols], banks[bank][0:64, 0:ncols], ALU.mult, (R_o, RB[bank]), (R_out,))

        m3 = A.mark()
        wkv_st = A.f32(1024)
        wkv = A.bf(1024)
        R_wkv = Res()
        P.dma("sp", wkv_st, w_ukv, (), (R_wkv,))
        P.copy("dve", wkv, wkv_st, (R_wkv,), (R_wkv,))
        ckvn = A.bf(SP)
        R_ck = Res()
        Kt = [A.bf(SP) for _ in range(2)]
        R_K = [Res(), Res()]
        Vt = [A.bf(128 * 65).rearrange("p (c f) -> p c f", f=65) for _ in range(2)]
        R_V = [Res(), Res()]
        Qh = [A.bf(NP_) for _ in range(2)]
        R_Q = [Res(), Res()]
        pT = [A.bf(1536) for _ in range(3)]
        R_pT = [Res() for _ in range(3)]
        o_sb = [A.f32(512) for _ in range(2)]
        R_osb = [Res(), Res()]
        rden = A.f32(512)
        R_rden = Res()
        ybf = [A.bf(512) for _ in range(2)]
        R_ybf = [Res(), Res()]
        for b in range(2):
            P.memset("pool", Vt[b][:, :, 64:65], 1.0, (R_V[b],))
        SCALE_A = 96.0 ** -0.5
        O_B = [6, 6]
        X_B = [7, 7]
        gcnt = {"st": 0, "o": 0, "x": 0, "y": 0, "pt": 0}
        for s in "ps":
            S_ = SEQ[s]
            n = NOWN[s]
            nkc = S_ // 128
            P.dma("sp", ckvn[:, 0:S_], ckvn_d[s], (R_ckvn[s],), (R_ck,))
            for b in range(2):
                P.dma("sp", Kt[b][64:96, 0:S_], kpe_d[s], (R_kpe[s],), (R_K[b],))
            def expand(h):
                kb = h % 2
                for t in range(S_ // 512):
                    xb = X_B[gcnt["x"] % 2]
                    gcnt["x"] += 1
                    P.mm(banks[xb][0:64, :], wkv[:, h * 128:h * 128 + 64], ckvn[:, t * 512:(t + 1) * 512], True, True,
                         (R_wkv, R_ck), (RB[xb],))
                    P.copy("dve", Kt[kb][0:64, t * 512:(t + 1) * 512], banks[xb][0:64, :], (RB[xb],), (R_K[kb],))
                    yield
                for t in range(S_ // 1024):
                    xb = X_B[gcnt["x"] % 2]
                    gcnt["x"] += 1
                    for c in range(8):
                        kc = t * 8 + c
                        P.mm(banks[xb][:, c * 64:(c + 1) * 64], ckvn[:, kc * 128:(kc + 1) * 128],
                             wkv[:, h * 128 + 64:h * 128 + 128], True, True, (R_wkv, R_ck), (RB[xb],))
                    P.copy("dve", Vt[kb][:, t * 8:(t + 1) * 8, 0:64], banks[xb].rearrange("p (c f) -> p c f", f=64),
                           (RB[xb],), (R_V[kb],))
                    yield

            for _ in expand(0):
                pass
            for h in range(8):
                kb = h % 2
                gen_next = expand(h + 1) if h + 1 < 8 else None
                nsteps = (S_ // 512 + S_ // 1024)
                tot_iters = (n // 512) * ((nkc + 2) // 3)
                every = max(1, tot_iters // (nsteps + 1))
                itc = 0
                P.dma("sp", Qh[kb][0:96, 0:n], qm_d[s][h], (R_qm[s],), (R_Q[kb],))
                for qt in range(n // 512):
                    ob = O_B[gcnt["o"] % 2]
                    gcnt["o"] += 1
                    qs = Qh[kb][0:96, qt * 512:(qt + 1) * 512]

                    groups = []
                    kc_ = 0
                    while kc_ < nkc:
                        g_ = min(3, nkc - kc_)
                        groups.append((kc_, g_))
                        kc_ += g_
                    ngr = len(groups)

                    def S_grp(gi):
                        k0, g_ = groups[gi]
                        sp_ = gcnt["st"] % 2
                        gcnt["st"] += 1
                        for u in range(g_):
                            kc = k0 + u
                            P.mm(banks[3 * sp_ + u], Kt[kb][0:96, kc * 128:(kc + 1) * 128], qs, True, True,
                                 (R_K[kb], R_Q[kb]), (RB[3 * sp_ + u],))
                        return sp_
                    sps = {0: S_grp(0), 1: S_grp(1)}
                    for k in range(ngr):
                        itc += 1
                        if gen_next is not None and itc % every == 0:
                            try:
                                next(gen_next)
                            except StopIteration:
                                gen_next = None
                        k0, g_ = groups[k]
                        sp_ = sps.pop(k)
                        pb = gcnt["pt"] % 3
                        gcnt["pt"] += 1
                        P.act(pT[pb][:, 0:g_ * 512], tri[sp_][:, 0:g_ * 512], AF.Exp,
                              tuple(RB[3 * sp_ + u] for u in range(g_)), (R_pT[pb],), scale=SCALE_A)
                        if k + 2 < ngr:
                            sps[k + 2] = S_grp(k + 2)
                        for u in range(g_):
                            kc = k0 + u
                            P.mm(banks[ob][0:65, :], Vt[kb][:, kc, :], pT[pb][:, u * 512:(u + 1) * 512], kc == 0, kc == nkc - 1,
                                 (R_V[kb], R_pT[pb]), (RB[ob],))
                    yb_ = gcnt["y"] % 2
                    gcnt["y"] += 1
                    P.copy("dve", o_sb[yb_][0:65, :], banks[ob][0:65, :], (RB[ob],), (R_osb[yb_],))
                    finalize(o_sb[yb_], 512, ybf[yb_][0:64, :], 7, R_osb[yb_], R_ybf[yb_], rden, R_rden)
                    P.dma("pool", ya_d[s][h * 64:(h + 1) * 64, qt * 512:(qt + 1) * 512], ybf[yb_][0:64, :],
                          (R_ybf[yb_],), (R_ya[s],))
                if gen_next is not None:
                    for _ in gen_next:
                        pass
        P.barrier()
        A.release(m3)

        m4 = A.mark()
        dmask32 = A.f32(48 * 128).rearrange("p (m f) -> p m f", f=128)
        dmask = A.bf(48 * 128).rearrange("p (m f) -> p m f", f=128)
        R_dm = Res()
        P.dma("sp", dmask32, dmask_in.rearrange("p (m f) -> p m f", f=128), (), (R_dm,))
        P.copy("dve", dmask, dmask32, (R_dm,), (R_dm,))
        NBLK = {s: [NOWN[s] // 128 + d for d in DPAT] for s in "ps"}
        NBT = sum(NBLK["p"])
        Vd = [A.bf(NBT * 130).rearrange("p (c f) -> p c f", f=130) for _ in range(2)]
        R_Vd = [Res(), Res()]
        qTd = [A.bf(NP_) for _ in range(2)]
        kTd = [A.bf(NP_ + 2 * HALO) for _ in range(2)]
        R_qk = [Res(), Res()]
        accd = [A.f32(NP_) for _ in range(2)]
        R_acc = [Res(), Res()]
        ebf = [A.bf(512) for _ in range(4)]
        R_ebf = [Res() for _ in range(4)]
        pTd = [A.bf(512) for _ in range(4)]
        R_pTd = [Res() for _ in range(4)]
        lnb = A.f32(512)
        rb4 = A.f32(512)
        R_fin4 = Res()
        ybf4 = [A.bf(512) for _ in range(2)]
        R_ybf4 = [Res(), Res()]
        SA_B = [0, 1, 2, 3]
        OD_B = [4, 5]
        c4 = {"o": 0, "y": 0, "hp": 0, "h": 0, "g": 0}
        items = []
        for s in "ps":
            n = NOWN[s]
            for hp in range(4):
                for hh in range(2):
                    for di, d in enumerate(DPAT):
                        pairs = []
                        if d == 1:
                            for qt in range(n // 128):
                                pairs.append((0, qt))
                        else:
                            for qt in range(n // (128 * d)):
                                for c in range(d):
                                    pairs.append((c, qt))
                        ng = len(pairs) // 4
                        for gi in range(ng):
                            items.append(dict(s=s, hp=hp, hh=hh, di=di, d=d, grp=pairs[gi * 4:gi * 4 + 4],
                                              first_pair=(hh == 0 and di == 0 and gi == 0),
                                              first_head=(di == 0 and gi == 0),
                                              last_head=(di == 2 and gi == ng - 1)))
        state = {}

        def stageA(it):
            s = it["s"]
            n = NOWN[s]
            d = it["d"]
            di = it["di"]
            hp = it["hp"]
            hh = it["hh"]
            h = hp * 2 + hh
            if it["first_pair"]:
                vb = c4["hp"] % 2
                c4["hp"] += 1
                state["vb"] = vb
                boff = []
                o_ = 0
                for d2 in DPAT:
                    boff.append(o_)
                    nb2 = n // (128 * d2) + 1
                    for c in range(d2):
                        r0 = c + HALO - 64 * d2
                        src = vd_d[s][r0:r0 + d2 * (128 * nb2 - 1) + 1:d2, hp * 130:(hp + 1) * 130]
                        src = src.rearrange("(k p) f -> p k f", p=128)
                        P.dma("sp", Vd[vb][:, o_ + c * nb2:o_ + (c + 1) * nb2, :], src, (R_vd[s],), (R_Vd[vb],))
                    o_ += nb2 * d2
                state["boff"] = boff
            if it["first_head"]:
                hb_ = c4["h"] % 2
                c4["h"] += 1
                state["hb"] = hb_
                P.dma("sp", qTd[hb_][0:64, 0:n], qd_d[s][h * 64:(h + 1) * 64, :], (R_qd[s],), (R_qk[hb_],))
                P.dma("sp", kTd[hb_][0:64, 0:n + 2 * HALO], kd_d[s][h * 64:(h + 1) * 64, :], (R_kd[s],), (R_qk[hb_],))
            hb_ = state["hb"]
            vb = state["vb"]
            g = c4["g"] % 2
            c4["g"] += 1
            sA, sBk = SA_B[2 * g], SA_B[2 * g + 1]
            for qi, (c, qt) in enumerate(it["grp"]):
                i0 = HALO // d + 128 * qt
                rq = c + d * i0 - HALO
                rhs = qTd[hb_][0:64, rq:rq + 127 * d + 1:d] if d > 1 else qTd[hb_][0:64, rq:rq + 128]
                for (bkk, ioff) in ((sA, i0 - 64), (sBk, i0 + 64)):
                    rk = c + d * ioff
                    lhs = kTd[hb_][0:64, rk:rk + 127 * d + 1:d] if d > 1 else kTd[hb_][0:64, rk:rk + 128]
                    P.mm(banks[bkk][:, qi * 128:(qi + 1) * 128], lhs, rhs, True, True, (R_qk[hb_],), (RB[bkk],))
            pts = []
            for ab, bkk in enumerate((sA, sBk)):
                eb = 2 * g + ab
                P.act(ebf[eb], banks[bkk], AF.Exp, (RB[bkk],), (R_ebf[eb],), scale=0.125)
                mk = dmask[:, (h * 3 + di) * 2 + ab, :].unsqueeze(1).broadcast_to([128, 4, 128])
                P.tt("dve", pTd[eb].rearrange("p (q f) -> p q f", f=128),
                     ebf[eb].rearrange("p (q f) -> p q f", f=128), mk, ALU.mult, (R_ebf[eb], R_dm), (R_pTd[eb],))
                pts.append(eb)
            return dict(hb=hb_, vb=vb, pts=pts, boff=state["boff"])

        def stageB(it, st):
            s = it["s"]
            n = NOWN[s]
            d = it["d"]
            di = it["di"]
            hh = it["hh"]
            h = it["hp"] * 2 + hh
            hb_, vb, pts, boff = st["hb"], st["vb"], st["pts"], st["boff"]
            nb = n // (128 * d) + 1
            acc = accd[hb_]
            ob = OD_B[c4["o"] % 2]
            c4["o"] += 1
            grp = it["grp"]
            for qi, (c, qt) in enumerate(grp):
                blkA = boff[di] + c * nb + qt
                for ab in range(2):
                    P.mm(banks[ob][0:65, qi * 128:(qi + 1) * 128], Vd[vb][:, blkA + ab, hh * 65:(hh + 1) * 65],
                         pTd[pts[ab]][:, qi * 128:(qi + 1) * 128], ab == 0, ab == 1,
                         (R_Vd[vb], R_pTd[pts[ab]]), (RB[ob],))
            c_0, qt0 = grp[0]
            if d == 1:
                av = acc[0:65, qt0 * 128:qt0 * 128 + 512]
                pv = banks[ob][0:65, :]
            else:
                base = 128 * d * qt0
                av = acc[0:65, base:base + 128 * d].rearrange("p (f c) -> p c f", c=d)[:, c_0:c_0 + 4, :]
                pv = banks[ob][0:65, :].rearrange("p (c f) -> p c f", f=128)
            if di == 0:
                P.copy("dve", av, pv, (RB[ob],), (R_acc[hb_],))
            else:
                P.tt("dve", av, av, pv, ALU.add, (RB[ob], R_acc[hb_]), (R_acc[hb_],))
            if it["last_head"]:
                for qt in range(n // 512):
                    yb_ = c4["y"] % 2
                    c4["y"] += 1
                    cs_ = slice(qt * 512, (qt + 1) * 512)
                    P.mm(banks[6][0:64, :], ones32[64:65, 0:64], acc[64:65, cs_], True, True, (R_acc[hb_], R_const), (RB[6],))
                    P.act(lnb[0:64, :], banks[6][0:64, :], AF.Ln, (RB[6],), (R_fin4,))
                    P.act(rb4[0:64, :], lnb[0:64, :], AF.Exp, (R_fin4,), (R_fin4,), scale=-1.0)
                    P.tt("dve", ybf4[yb_][0:64, :], acc[0:64, cs_], rb4[0:64, :], ALU.mult, (R_acc[hb_], R_fin4), (R_ybf4[yb_],))
                    P.dma("pool", yb_d[s][h * 64:(h + 1) * 64, qt * 512:(qt + 1) * 512], ybf4[yb_][0:64, :],
                          (R_ybf4[yb_],), (R_yb[s],))

        sts = {0: stageA(items[0])}
        for i in range(len(items)):
            if i + 1 < len(items):
                sts[i + 1] = stageA(items[i + 1])
            stageB(items[i], sts.pop(i))
        P.barrier()
        A.release(m4)

        m5 = A.mark()
        TB = 1024
        R_w5 = Res()
        wob = A.bf(8 * 1024).rearrange("p (c n) -> p c n", n=1024)
        wr32 = A.f32(8 * 20).rearrange("p (j c) -> p j c", c=20)
        selb = A.bf(NEXP * 128, 16)
        NSTG = 4
        wstg = [A.f32(4 * 512) for _ in range(NSTG)]
        R_wstg = [Res() for _ in range(NSTG)]
        sc5 = {"stg": 0}

        def load_cast(dst_bf, src_ap, shape_c, eng):
            k_ = sc5["stg"] % NSTG
            sc5["stg"] += 1
            jj, cc = shape_c
            st = wstg[k_][:, 0:jj * cc].rearrange("p (j c) -> p j c", c=cc)
            P.dma("sp", st, src_ap, (), (R_wstg[k_],))
            return k_, st

        for half in range(4):
            k_, st = load_cast(None, w_out.rearrange("(c p) n -> p c n", p=128)[:, half * 2:(half + 1) * 2, :], (2, 1024), "dve")
            P.copy("dve", wob[:, half * 2:(half + 1) * 2, :], st, (R_wstg[k_],), (R_w5,))
        P.dma("sp", wr32, w_r.rearrange("(j p) c -> p j c", p=128), (), (R_w5,))
        k_ = sc5["stg"] % NSTG
        sc5["stg"] += 1
        P.dma("sp", wstg[k_][0:16, 0:NEXP * 128], sel_in, (), (R_wstg[k_],))
        P.copy("dve", selb, wstg[k_][0:16, 0:NEXP * 128], (R_wstg[k_],), (R_w5,))

        acc5 = A.f32(8 * TB).rearrange("p (j c) -> p j c", c=TB)
        R_acc5 = Res()
        h2b = A.bf(8 * TB).rearrange("p (j c) -> p j c", c=TB)
        R_h2b = Res()
        h2f = A.f32(8 * 512).rearrange("p (j c) -> p j c", c=512)
        R_h2f = Res()
        yab = A.bf(8 * 512).rearrange("p (c n) -> p c n", n=512)
        R_yab = Res()
        sq5 = yab
        R_sq5 = R_yab
        rstd5 = A.f32(512)
        lnt5 = A.f32(512)
        R_rstd5 = Res()
        wcT = A.bf(TB, 16)
        R_wcT = Res()
        wg = [A.bf(8 * 512).rearrange("p (j c) -> p j c", c=512) for _ in range(2)]
        wu = [A.bf(8 * 512).rearrange("p (j c) -> p j c", c=512) for _ in range(2)]
        wd = [A.bf(4 * 1024).rearrange("p (j c) -> p j c", c=1024) for _ in range(2)]
        R_wg = [Res(), Res()]
        R_wu = [Res(), Res()]
        R_wd = [Res(), Res()]
        wcb = [A.bf(512) for _ in range(2)]
        R_wcb = [Res(), Res()]
        sa = [A.bf(512) for _ in range(2)]
        R_sa = [Res(), Res()]
        uw = [A.bf(512) for _ in range(2)]
        R_uw = [Res(), Res()]
        hid = [A.bf(4 * 512).rearrange("p (j c) -> p j c", c=512) for _ in range(2)]
        R_hid = [Res(), Res()]
        rl = A.f32(80).rearrange("p (q c) -> p q c", c=20)
        r_a = A.f32(16).rearrange("p (q c) -> p q c", c=4)
        goh = A.f32(16).rearrange("p (q c) -> p q c", c=4)
        r_m = A.f32(4)
        r_s = A.f32(4)
        gw = A.f32(4)
        esel = A.f32(64).rearrange("p (q g e) -> p q g e", g=4, e=4)
        el = A.f32(16).rearrange("p (q c) -> p q c", c=4)
        ee = A.f32(16).rearrange("p (q c) -> p q c", c=4)
        mk1 = A.f32(16).rearrange("p (q c) -> p q c", c=4)
        ee2 = A.f32(16).rearrange("p (q c) -> p q c", c=4)
        mk2 = A.f32(16).rearrange("p (q c) -> p q c", c=4)
        m1_ = A.f32(4)
        m2_ = A.f32(4)
        wexp = A.f32(16).rearrange("p (q c) -> p q c", c=4)
        wc = A.f32(64).rearrange("p (q g e) -> p q g e", g=4, e=4)
        R_rt = Res()
        yout = [A.f32(512) for _ in range(2)]
        R_yout = [Res(), Res()]

        def bc4(ap):
            return ap.unsqueeze(2).broadcast_to([128, 4, 4])

        c5 = {"pb": 0, "e": 0, "ob": 0, "y": 0, "w": 0}
        PB5 = [0, 1, 2, 3]
        OB5 = [4, 5]
        blocks = []
        for s in "ps":
            for bi in range(NOWN[s] // TB):
                blocks.append((s, bi))
        PIECES = []
        for gb in range(len(blocks)):
            for e in range(NEXP):
                wb_ = (gb * NEXP + e) % 2
                for hf in range(2):
                    PIECES.append((w_gate[e].rearrange("(j p) c -> p j c", p=128)[:, hf * 4:(hf + 1) * 4, :],
                                   wg[wb_][:, hf * 4:(hf + 1) * 4, :], R_wg[wb_], 512))
                for hf in range(2):
                    PIECES.append((w_up[e].rearrange("(j p) c -> p j c", p=128)[:, hf * 4:(hf + 1) * 4, :],
                                   wu[wb_][:, hf * 4:(hf + 1) * 4, :], R_wu[wb_], 512))
                for hf in range(2):
                    PIECES.append((w_down[e].rearrange("(j p) c -> p j c", p=128)[:, hf * 2:(hf + 1) * 2, :],
                                   wd[wb_][:, hf * 2:(hf + 1) * 2, :], R_wd[wb_], 1024))
        pc = {"dma": 0, "cast": 0}
        pstg = {}

        def piece_dma():
            i = pc["dma"]
            if i >= len(PIECES):
                return
            pc["dma"] += 1
            src, dst, Rd, cc = PIECES[i]
            k_ = sc5["stg"] % NSTG
            sc5["stg"] += 1
            st = wstg[k_].rearrange("p (j c) -> p j c", c=cc)
            P.dma("sp", st, src, (), (R_wstg[k_],))
            pstg[i] = (k_, st)

        def piece_cast():
            i = pc["cast"]
            if i >= len(PIECES):
                return
            pc["cast"] += 1
            src, dst, Rd, cc = PIECES[i]
            k_, st = pstg.pop(i)
            P.copy("act", dst, st, (R_wstg[k_],), (Rd,))

        def slot():
            piece_cast()
            piece_dma()

        for _ in range(3):
            piece_dma()
        for _ in range(6):
            slot()
        ucnt = {"u": 0}
        for gb, (s, bi) in enumerate(blocks):
            si = SIDX[s]
            for tl in range(TB // 512):
                q0 = bi * TB + tl * 512
                tc0 = tl * 512
                P.dma("sp", acc5[:, :, tc0:tc0 + 512],
                      xT[s].rearrange("(j p) c -> p j c", p=128)[:, :, HALO + q0:HALO + q0 + 512], (), (R_acc5,))
                P.dma("sp", yab[:, 0:4, :], ya_d[s].rearrange("(c p) n -> p c n", p=128)[:, :, q0:q0 + 512], (R_ya[s],), (R_yab,))
                P.dma("sp", yab[:, 4:8, :], yb_d[s].rearrange("(c p) n -> p c n", p=128)[:, :, q0:q0 + 512], (R_yb[s],), (R_yab,))
                for j in range(8):
                    bk = PB5[c5["pb"] % 4]
                    c5["pb"] += 1
                    for c in range(8):
                        P.mm(banks[bk], wob[:, c, j * 128:(j + 1) * 128], yab[:, c, :], c == 0, c == 7, (R_w5, R_yab), (RB[bk],))
                    P.stt("dve", acc5[:, j, tc0:tc0 + 512], banks[bk], mod[:, 16 + j, si:si + 1], acc5[:, j, tc0:tc0 + 512],
                          ALU.mult, ALU.add, (RB[bk], R_acc5, R_mod), (R_acc5,))
                P.act(sq5, acc5[:, :, tc0:tc0 + 512], AF.Square, (R_acc5,), (R_sq5,))
                for j in range(8):
                    P.mm(banks[6], ones_bf, sq5[:, j, :], j == 0, j == 7, (R_sq5, R_const), (RB[6],))
                rstd_from_ss(rstd5, banks[6], 1.0 / D, (RB[6], R_const), (R_rstd5,), lnt5)
                for j in range(8):
                    P.stt("dve", h2f[:, j, :], acc5[:, j, tc0:tc0 + 512], g2s[:, j, si:si + 1], rstd5, ALU.mult, ALU.mult,
                          (R_acc5, R_rstd5, R_mod), (R_h2f,))
                for j in range(8):
                    P.act(h2f[:, j, :], h2f[:, j, :], AF.Identity, (R_h2f, R_mod), (R_h2f,), bias=mod[:, 24 + j, si:si + 1])
                for j in range(8):
                    P.copy("dve", h2b[:, j, tc0:tc0 + 512], h2f[:, j, :], (R_h2f,), (R_h2b,))
                for q in range(4):
                    for j in range(8):
                        P.mm(banks[7][:, q * 20:(q + 1) * 20], h2f[:, j, q * 128:(q + 1) * 128], wr32[:, j, :], j == 0, j == 7,
                             (R_h2f, R_w5), (RB[7],))
                P.copy("dve", rl, banks[7][:, 0:80].rearrange("p (q c) -> p q c", c=20), (RB[7],), (R_rt,))
                RT = (R_rt,)
                gl = rl[:, :, 0:4]
                P.reduce(r_m, gl, ALU.max, RT, RT)
                P.tt("dve", r_a, gl, bc4(r_m), ALU.subtract, RT, RT)
                P.tt("dve", goh, gl, bc4(r_m), ALU.is_equal, RT, RT)
                P.act(r_a, r_a, AF.Exp, RT, RT)
                P.reduce(r_s, r_a, ALU.add, RT, RT)
                P.recip(gw, r_s, RT, RT)
                P.tt("dve", esel, rl[:, :, 4:20].rearrange("p q (g e) -> p q g e", e=4),
                     goh.unsqueeze(3).broadcast_to([128, 4, 4, 4]), ALU.mult, RT, RT)
                P.reduce(el, esel.rearrange("p q g e -> p q e g"), ALU.add, RT, RT)
                P.reduce(r_m, el, ALU.max, RT, RT)
                P.tt("dve", ee, el, bc4(r_m), ALU.subtract, RT, RT)
                P.act(ee, ee, AF.Exp, RT, RT)
                P.reduce(m1_, ee, ALU.max, RT, RT)
                P.tt("dve", mk1, ee, bc4(m1_), ALU.is_equal, RT, RT)
                P.tt("dve", ee2, ee, mk1, ALU.mult, RT, RT)
                P.tt("dve", ee2, ee, ee2, ALU.subtract, RT, RT)
                P.reduce(m2_, ee2, ALU.max, RT, RT)
                P.tt("dve", mk2, ee2, bc4(m2_), ALU.is_equal, RT, RT)
                P.tt("dve", mk1, mk1, mk2, ALU.add, RT, RT)
                P.tt("dve", m1_, m1_, m2_, ALU.add, RT, RT)
                P.recip(m1_, m1_, RT, RT)
                P.tt("dve", m1_, m1_, gw, ALU.mult, RT, RT)
                P.tt("dve", wexp, ee, mk1, ALU.mult, RT, RT)
                P.tt("dve", wexp, wexp, bc4(m1_), ALU.mult, RT, RT)
                P.tt("dve", wc, goh.unsqueeze(3).broadcast_to([128, 4, 4, 4]),
                     wexp.unsqueeze(2).broadcast_to([128, 4, 4, 4]), ALU.mult, RT, RT)
                for q in range(4):
                    P.tr(banks[6][0:16, q * 128:(q + 1) * 128],
                         wc[:, q].rearrange("p g e -> p (g e)"), ident, (R_rt, R_const), (RB[6],))
                P.copy("dve", wcT[0:16, tc0:tc0 + 512], banks[6][0:16, :], (RB[6],), (R_wcT,))
            units = []
            for e in range(NEXP):
                for tl in range(TB // 512):
                    units.append((e, tl, (gb * NEXP + e) % 2))

            def GU(u):
                e, tl, wbuf = u
                tc0 = tl * 512
                cb = ucnt["u"] % 2
                ucnt["u"] += 1
                P.mm(banks[7], selb[0:16, e * 128:(e + 1) * 128], wcT[0:16, tc0:tc0 + 512], True, True,
                     (R_w5, R_wcT), (RB[7],))
                P.copy("act", wcb[cb], banks[7], (RB[7],), (R_wcb[cb],))
                for fc in range(4):
                    ba = PB5[c5["pb"] % 4]
                    bu = PB5[(c5["pb"] + 1) % 4]
                    c5["pb"] += 2
                    for j in range(8):
                        P.mm(banks[ba], wg[wbuf][:, j, fc * 128:(fc + 1) * 128], h2b[:, j, tc0:tc0 + 512], j == 0, j == 7,
                             (R_wg[wbuf], R_h2b), (RB[ba],))
                    for j in range(8):
                        P.mm(banks[bu], wu[wbuf][:, j, fc * 128:(fc + 1) * 128], h2b[:, j, tc0:tc0 + 512], j == 0, j == 7,
                             (R_wu[wbuf], R_h2b), (RB[bu],))
                    sb_ = fc % 2
                    P.act(sa[sb_], banks[ba], AF.Silu, (RB[ba],), (R_sa[sb_],))
                    P.tt("dve", uw[sb_], banks[bu], wcb[cb], ALU.mult, (RB[bu], R_wcb[cb]), (R_uw[sb_],))
                    P.tt("dve", hid[cb][:, fc, :], sa[sb_], uw[sb_], ALU.mult, (R_sa[sb_], R_uw[sb_]), (R_hid[cb],))
                    if fc < 3:
                        slot()
                return cb

            def DN(u, cb):
                e, tl, wbuf = u
                tc0 = tl * 512
                for j in range(8):
                    ob = OB5[c5["ob"] % 2]
                    c5["ob"] += 1
                    for fc in range(4):
                        P.mm(banks[ob], wd[wbuf][:, fc, j * 128:(j + 1) * 128], hid[cb][:, fc, :], fc == 0, fc == 3,
                             (R_wd[wbuf], R_hid[cb]), (RB[ob],))
                    P.stt("dve", acc5[:, j, tc0:tc0 + 512], banks[ob], mod[:, 40 + j, si:si + 1], acc5[:, j, tc0:tc0 + 512],
                          ALU.mult, ALU.add, (RB[ob], R_acc5, R_mod), (R_acc5,))

            cbs = {0: GU(units[0])}
            for i in range(len(units)):
                if i + 1 < len(units):
                    cbs[i + 1] = GU(units[i + 1])
                DN(units[i], cbs.pop(i))
            for tl in range(TB // 512):
                tc0 = tl * 512
                q0 = bi * TB + tc0
                P.act(sq5, acc5[:, :, tc0:tc0 + 512], AF.Square, (R_acc5,), (R_sq5,))
                for j in range(8):
                    P.mm(banks[6], ones_bf, sq5[:, j, :], j == 0, j == 7, (R_sq5, R_const), (RB[6],))
                rstd_from_ss(rstd5, banks[6], 1.0 / D, (RB[6], R_const), (R_rstd5,), lnt5)
                for j in range(8):
                    yb_ = c5["y"] % 2
                    c5["y"] += 1
                    P.stt("dve", yout[yb_], acc5[:, j, tc0:tc0 + 512], g_fin[:, j:j + 1], rstd5, ALU.mult, ALU.mult,
                          (R_acc5, R_rstd5, R_vec), (R_yout[yb_],))
                    P.dma("pool", yT[s][j * 128:(j + 1) * 128, q0:q0 + 512], yout[yb_], (R_yout[yb_],), ())
        A.release(m5)

        with nc.Block() as block:
            P.emit(block, csem, dsem)
    return nc


def _rope_tables(S):
    inv = (np.float32(10000.0) ** (-np.arange(0, 32, 2, dtype=np.float32) / np.float32(32))).astype(np.float32)
    ang = (np.arange(S, dtype=np.float32)[:, None] * inv[None, :]).astype(np.float32)
    cos = np.cos(ang).astype(np.float32).T
    sin = np.sin(ang).astype(np.float32).T
    cs = np.concatenate([cos, cos], axis=0)
    sn = np.concatenate([-sin, sin], axis=0)
    return cs, sn


def _dmask():
    slopes = (2.0 ** (-8.0 * np.arange(1, 9) / 8)).astype(np.float32)
    p = np.arange(128)[:, None]
    f = np.arange(128)[None, :]
    out = np.zeros((128, 48, 128), np.float32)
    for h in range(8):
        for di, d in enumerate(DPAT):
            dA = p - 64 - f
            mA = np.where(p >= f, np.exp(-slopes[h] * np.abs(dA).astype(np.float32) * d), 0.0)
            dB = p + 64 - f
            mB = np.where(p <= f, np.exp(-slopes[h] * np.abs(dB).astype(np.float32) * d), 0.0)
            out[:, (h * 3 + di) * 2 + 0, :] = mA
            out[:, (h * 3 + di) * 2 + 1, :] = mB
    return out.reshape(128, 48 * 128)


_NC_CACHE = {}


def kernel(x_prompt, x_sample, c_prompt, c_sample, ada_w, ada_b, norm_mix_g, w_in, q_norm_g, kv_norm_g,
           w_uq, w_ukv, w_out, norm_moe_g, w_router_group, w_router_expert, w_gate, w_up, w_down,
           final_norm_g, _debug_out=None):
    f = np.float32
    x_prompt = np.asarray(x_prompt, f)
    x_sample = np.asarray(x_sample, f)
    w_in0 = np.ascontiguousarray(np.asarray(w_in, f)[0])
    w_uq0 = np.asarray(w_uq, f)[0]
    pe = w_in0[:, 384:416]
    w_pe_sw = np.ascontiguousarray(np.concatenate([pe[:, 16:32], pe[:, 0:16]], axis=1))
    wq = w_uq0.reshape(256, 8, 96)
    wq_sw = wq.copy()
    wq_sw[:, :, 64:80] = wq[:, :, 80:96]
    wq_sw[:, :, 80:96] = wq[:, :, 64:80]
    w_uq_sw = np.ascontiguousarray(wq_sw.reshape(256, 768))
    w_r = np.ascontiguousarray(np.concatenate([np.asarray(w_router_group, f)[0],
                                               np.asarray(w_router_expert, f)[0].reshape(D, 16)], axis=1))
    cs_full = {"p": _rope_tables(SP), "s": _rope_tables(SS)}
    dmask = _dmask()
    ident = np.eye(128, dtype=f)
    sel = np.zeros((16, NEXP * 128), f)
    for e in range(NEXP):
        sel[e, e * 128:(e + 1) * 128] = 1.0
    shared = {
        "ada_w": np.ascontiguousarray(np.asarray(ada_w, f)[0]),
        "w_in": w_in0, "w_pe_sw": w_pe_sw,
        "w_uq": np.ascontiguousarray(w_uq0), "w_uq_sw": w_uq_sw,
        "w_ukv": np.ascontiguousarray(np.asarray(w_ukv, f)[0]),
        "w_out": np.ascontiguousarray(np.asarray(w_out, f)[0]),
        "w_r": w_r,
        "w_gate": np.ascontiguousarray(np.asarray(w_gate, f)[0]),
        "w_up": np.ascontiguousarray(np.asarray(w_up, f)[0]),
        "w_down": np.ascontiguousarray(np.asarray(w_down, f)[0]),
        "dmask": dmask, "ident": ident, "sel": sel,
    }

    def chunked(v, nch):
        return np.asarray(v, f).reshape(nch, 128).T

    xT_full = {}
    for g in range(2):
        xT_full[("p", g)] = np.ascontiguousarray(x_prompt[g].T)
        xT_full[("s", g)] = np.ascontiguousarray(x_sample[g].T)
    in_maps = []
    for core in range(8):
        g, j = core // 4, core % 4
        m = dict(shared)
        c2 = np.stack([np.asarray(c_prompt, f)[g], np.asarray(c_sample, f)[g]], axis=1)
        vecs = np.zeros((128, 96), f)
        vecs[:, 0:16] = c2.reshape(8, 128, 2).transpose(1, 0, 2).reshape(128, 16)
        vecs[:, 16:64] = chunked(np.asarray(ada_b, f)[0], 48)
        vecs[:, 64:72] = chunked(np.asarray(norm_mix_g, f)[0], 8)
        vecs[:, 72:80] = chunked(np.asarray(norm_moe_g, f)[0], 8)
        vecs[:, 80:88] = chunked(np.asarray(final_norm_g, f), 8)
        vecs[:, 88:90] = chunked(np.asarray(q_norm_g, f)[0], 2)
        vecs[:, 90:91] = chunked(np.asarray(kv_norm_g, f)[0], 1)
        m["vecs"] = vecs
        for s, S_, n in (("p", SP, NP_), ("s", SS, NS_)):
            o = j * n
            idx = (o - HALO + np.arange(S_)) % S_
            m["xT" + s] = np.ascontiguousarray(xT_full[(s, g)][:, idx])
            cs, sn = cs_full[s]
            m["cs_" + s] = np.ascontiguousarray(cs[:, idx])
            m["sn_" + s] = np.ascontiguousarray(sn[:, idx])
            u = o - HALO + np.arange(n + 2 * HALO)
            val = ((u >= 0) & (u < S_)).astype(f)
            m["valid_" + s] = np.ascontiguousarray(val.reshape(-1, 128).T)
        in_maps.append(m)

    if "nc" not in _NC_CACHE:
        _NC_CACHE["nc"] = build_program()
    nc = _NC_CACHE["nc"]
    res = run_bass_kernel_spmd(nc, in_maps, core_ids=list(range(8)))
    y_prompt = np.empty((2, SP, D), f)
    y_sample = np.empty((2, SS, D), f)
    for core in range(8):
        g, j = core // 4, core % 4
        r = res.results[core]
        y_prompt[g, j * NP_:(j + 1) * NP_, :] = np.asarray(r["yTp"]).T
        y_sample[g, j * NS_:(j + 1) * NS_, :] = np.asarray(r["yTs"]).T
    if _debug_out is not None:
        _debug_out.append(res)
    return (y_prompt, y_sample)


if __name__ == "__main__":
    import time
    t = time.time()
    nc = build_program()
    print("build ok", time.time() - t)
```

```python
import numpy as np
import concourse.bass as bass
import concourse.mybir as mybir
from concourse.bass_utils import run_bass_kernel_spmd

F32 = mybir.dt.float32
BF16 = mybir.dt.bfloat16
AF = mybir.ActivationFunctionType
ALU = mybir.AluOpType
AX = mybir.AxisListType

DEBUG = False
STRICT_SAME_ENGINE = True

D = 1024
SP, SS = 16384, 8192
NP_, NS_ = 4096, 2048
HALO = 1024
EPS = 1e-6
NEXP = 16
DPAT = (1, 4, 16)


class Res:
    __slots__ = ("name", "w", "r")

    def __init__(self, name=""):
        self.name = name
        self.w = {}
        self.r = {}


class Prog:
    CENG = ("pe", "act", "dve", "pool")

    def __init__(self, nc):
        self.nc = nc
        self.engs = ["pe", "act", "dve", "pool", "sp"]
        self.ops = {e: [] for e in self.engs}
        self.pending = {e: [] for e in self.engs}
        self.nslots = {"sp": 12, "pool": 8, "act": 4}
        self.dma_count = {q: 0 for q in self.nslots}
        self.dma_tok = {q: [None] * n for q, n in self.nslots.items()}

    def _collect(self, eng, reads, writes, is_dma=False):
        waits = list(self.pending[eng])
        self.pending[eng] = []
        for r in reads:
            for t in r.w.values():
                waits.append(t)
        for w in writes:
            for t in w.w.values():
                if t[0] == "c" and t[1] == eng and not STRICT_SAME_ENGINE:
                    continue
                if is_dma and t[0] == "d":
                    continue
                waits.append(t)
            for t in w.r.values():
                if t[0] == "c" and t[1] == eng and not STRICT_SAME_ENGINE:
                    continue
                waits.append(t)
        if eng == "pe":
            waits = [t for t in waits if not (t[0] == "c" and t[1] == "pe")]
        return waits

    def op(self, eng, fn, reads=(), writes=()):
        waits = self._collect(eng, reads, writes)
        idx = len(self.ops[eng])
        tok = ("c", eng, idx)
        self.ops[eng].append(dict(fn=fn, waits=waits, kind="c"))
        for r in reads:
            r.r[eng] = tok
        for w in writes:
            w.w[eng] = tok
        return tok

    def dma(self, q, out, in_, reads=(), writes=()):
        waits = self._collect(q, reads, writes, is_dma=True)
        k = self.dma_count[q]
        ns = self.nslots[q]
        slot = k % ns
        value = 16 * (k // ns + 1)
        self.dma_count[q] = k + 1
        if self.dma_tok[q][slot] is not None:
            waits.append(self.dma_tok[q][slot])
        tok = ("d", q, slot, value)
        self.dma_tok[q][slot] = tok
        self.ops[q].append(dict(fn=lambda e: e.dma_start(out=out, in_=in_), waits=waits, kind="d", q=q, slot=slot))
        key = (q, slot)
        for r in reads:
            r.r[key] = tok
        for w in writes:
            w.w[key] = tok
        return tok

    def barrier(self):
        toks = []
        for e in self.CENG:
            n = len(self.ops[e])
            for i in range(n - 1, -1, -1):
                if self.ops[e][i]["kind"] == "c":
                    toks.append(("c", e, i))
                    break
        for q in self.nslots:
            for t in self.dma_tok[q]:
                if t is not None:
                    toks.append(t)
        for e in self.engs:
            self.pending[e].extend(toks)

    def emit(self, block, csem, dsem):
        needed = {e: set() for e in self.CENG}
        for e in self.engs:
            for o in self.ops[e]:
                for t in o["waits"]:
                    if t[0] == "c":
                        needed[t[1]].add(t[2])
        final = []
        for q in self.nslots:
            for t in self.dma_tok[q]:
                if t is not None:
                    final.append(t)
        rank = {e: {idx: i + 1 for i, idx in enumerate(sorted(needed[e]))} for e in self.CENG}

        def body(ename):
            def f(eh):
                waited = {}

                def do_wait(t):
                    if t[0] == "c":
                        sem = csem[t[1]]
                        val = rank[t[1]][t[2]]
                        key = t[1]
                    else:
                        sem = dsem[t[1]][t[2]]
                        val = t[3]
                        key = (t[1], t[2])
                    if waited.get(key, 0) >= val:
                        return
                    eh.wait_ge(sem, val)
                    waited[key] = val

                for idx, o in enumerate(self.ops[ename]):
                    for t in o["waits"]:
                        do_wait(t)
                    ins = o["fn"](eh)
                    if o["kind"] == "c":
                        if ename in rank and idx in rank[ename]:
                            ins.then_inc(csem[ename], 1)
                    else:
                        ins.then_inc(dsem[o["q"]][o["slot"]], 16)
                if ename == "sp":
                    for t in final:
                        do_wait(t)
            return f

        block.tensor(body("pe"))
        block.scalar(body("act"))
        block.vector(body("dve"))
        block.gpsimd(body("pool"))
        block.sync(body("sp"))

    def mm(self, out, lhsT, rhs, start, stop, R, W):
        self.op("pe", lambda e: e.matmul(out, lhsT=lhsT, rhs=rhs, start=start, stop=stop), R, W)

    def tr(self, out, in_, ident, R, W):
        self.op("pe", lambda e: e.transpose(out, in_, ident), R, W)

    def act(self, out, in_, func, R, W, bias=None, scale=None):
        kw = {}
        if bias is not None:
            kw["bias"] = bias
        if scale is not None:
            kw["scale"] = scale
        self.op("act", lambda e: e.activation(out=out, in_=in_, func=func, **kw), R, W)

    def tt(self, eng, out, in0, in1, op, R, W):
        self.op(eng, lambda e: e.tensor_tensor(out=out, in0=in0, in1=in1, op=op), R, W)

    def ts(self, eng, out, in0, s1, op0, R, W, s2=None, op1=None):
        if op1 is None:
            self.op(eng, lambda e: e.tensor_scalar(out=out, in0=in0, scalar1=s1, scalar2=None, op0=op0), R, W)
        else:
            self.op(eng, lambda e: e.tensor_scalar(out=out, in0=in0, scalar1=s1, scalar2=s2, op0=op0, op1=op1), R, W)

    def stt(self, eng, out, in0, scalar, in1, op0, op1, R, W):
        self.op(eng, lambda e: e.scalar_tensor_tensor(out=out, in0=in0, scalar=scalar, in1=in1, op0=op0, op1=op1), R, W)

    def copy(self, eng, out, in_, R, W):
        if eng == "act":
            self.op("act", lambda e: e.copy(out=out, in_=in_), R, W)
        else:
            self.op(eng, lambda e: e.tensor_copy(out=out, in_=in_), R, W)

    def memset(self, eng, ap, val, W):
        self.op(eng, lambda e: e.memset(ap, val), (), W)

    def recip(self, out, in_, R, W):
        self.op("dve", lambda e: e.reciprocal(out=out, in_=in_), R, W)

    def reduce(self, out, in_, op, R, W):
        self.op("dve", lambda e: e.tensor_reduce(out=out, in_=in_, axis=AX.X, op=op), R, W)


class Arena:
    def __init__(self, t, nfloats):
        self.t = t
        self.n = nfloats * 4
        self.off = 0

    def mark(self):
        return self.off

    def release(self, m):
        self.off = m

    def f32(self, n, parts=128):
        o = (self.off + 31) // 32 * 32
        assert o + 4 * n <= self.n, f"arena overflow {o + 4 * n} > {self.n}"
        self.off = o + 4 * n
        return self.t[0:parts, o // 4:o // 4 + n]

    def bf(self, n, parts=128):
        nn = (n + 1) // 2
        return self.f32(nn, parts).bitcast(BF16)[:, 0:n]


def build_program():
    nc = bass.Bass("TRN2", target_bir_lowering=False)
    skind = "ExternalOutput" if DEBUG else "Internal"

    def din(name, shape, dt=F32):
        return nc.dram_tensor(name, list(shape), dt, kind="ExternalInput").ap()

    def dscr(name, shape, dt=BF16):
        return nc.dram_tensor(name, list(shape), dt, kind=skind).ap()

    xT = {"p": din("xTp", [D, SP]), "s": din("xTs", [D, SS])}
    vecs = din("vecs", [128, 96])
    ada_w = din("ada_w", [D, 6 * D])
    w_in = din("w_in", [D, 1952])
    w_pe_sw = din("w_pe_sw", [D, 32])
    w_uq = din("w_uq", [256, 768])
    w_uq_sw = din("w_uq_sw", [256, 768])
    w_ukv = din("w_ukv", [128, 1024])
    w_out = din("w_out", [D, D])
    w_r = din("w_r", [D, 20])
    w_gate = din("w_gate", [NEXP, D, 512])
    w_up = din("w_up", [NEXP, D, 512])
    w_down = din("w_down", [NEXP, 512, D])
    cs_in = {"p": din("cs_p", [32, SP]), "s": din("cs_s", [32, SS])}
    sn_in = {"p": din("sn_p", [32, SP]), "s": din("sn_s", [32, SS])}
    valid_in = {"p": din("valid_p", [128, 48]), "s": din("valid_s", [128, 32])}
    dmask_in = din("dmask", [128, 48 * 128])
    ident_in = din("ident", [128, 128])
    sel_in = din("sel", [16, NEXP * 128])

    yT = {"p": nc.dram_tensor("yTp", [D, NP_], F32, kind="ExternalOutput").ap(),
          "s": nc.dram_tensor("yTs", [D, NS_], F32, kind="ExternalOutput").ap()}

    SEQ = {"p": SP, "s": SS}
    NOWN = {"p": NP_, "s": NS_}
    SIDX = {"p": 0, "s": 1}
    ckvn_d = {s: dscr("ckvn_" + s, [128, SEQ[s]]) for s in "ps"}
    kpe_d = {s: dscr("kpe_" + s, [32, SEQ[s]]) for s in "ps"}
    qm_d = {s: dscr("qm_" + s, [8, 96, NOWN[s]]) for s in "ps"}
    qd_d = {s: dscr("qd_" + s, [512, NOWN[s]]) for s in "ps"}
    kd_d = {s: dscr("kd_" + s, [512, NOWN[s] + 2 * HALO]) for s in "ps"}
    vd_d = {s: dscr("vd_" + s, [NOWN[s] + 2 * HALO, 520]) for s in "ps"}
    ya_d = {s: dscr("ya_" + s, [512, NOWN[s]]) for s in "ps"}
    yb_d = {s: dscr("yb_" + s, [512, NOWN[s]]) for s in "ps"}
    R_ckvn = {s: Res() for s in "ps"}
    R_kpe = {s: Res() for s in "ps"}
    R_qm = {s: Res() for s in "ps"}
    R_qd = {s: Res() for s in "ps"}
    R_kd = {s: Res() for s in "ps"}
    R_vd = {s: Res() for s in "ps"}
    R_ya = {s: Res() for s in "ps"}
    R_yb = {s: Res() for s in "ps"}

    NARENA = 52000
    from contextlib import ExitStack
    with ExitStack() as es:
        arena_t = es.enter_context(nc.sbuf_tensor("arena", [128, NARENA], F32))
        tri = [es.enter_context(nc.psum_tensor(f"tri{i}", [128, 1536], F32)) for i in range(2)]
        tri = [p_[:, :] for p_ in tri]
        pair_ = es.enter_context(nc.psum_tensor("pair", [128, 1024], F32))[:, :]
        banks = [tri[i // 3][:, (i % 3) * 512:(i % 3 + 1) * 512] for i in range(6)]
        banks += [pair_[:, 0:512], pair_[:, 512:1024]]
        RB = [Res(f"bank{i}") for i in range(8)]
        P = Prog(nc)
        csem = {e: es.enter_context(nc.semaphore("c_" + e)) for e in Prog.CENG}
        dsem = {q: [es.enter_context(nc.semaphore(f"d_{q}{i}")) for i in range(n)] for q, n in P.nslots.items()}
        A = Arena(arena_t, NARENA)

        vec = A.f32(96)
        R_vec = Res()
        P.dma("sp", vec, vecs, (), (R_vec,))
        cT = vec[:, 0:16].rearrange("p (j s) -> p j s", s=2)
        ada_b = vec[:, 16:64]
        g_mix = vec[:, 64:72]
        g_moe = vec[:, 72:80]
        g_fin = vec[:, 80:88]
        qg = vec[:, 88:90]
        kvg = vec[:, 90:91]
        ones_bf = A.bf(128)
        ones32 = A.f32(64)
        R_const = Res()
        P.memset("dve", ones_bf, 1.0, (R_const,))
        P.memset("dve", ones32, 1.0, (R_const,))
        ident = A.f32(128)
        P.dma("sp", ident, ident_in, (), (R_const,))
        mod = A.f32(96).rearrange("p (m s) -> p m s", s=2)
        g1s = A.f32(16).rearrange("p (j s) -> p j s", s=2)
        g2s = A.f32(16).rearrange("p (j s) -> p j s", s=2)
        R_mod = Res()

        m0 = A.mark()
        sc = A.f32(16).rearrange("p (j s) -> p j s", s=2)
        R_sc = Res()
        P.act(sc, cT, AF.Silu, (R_vec,), (R_sc,))
        adaw_v = ada_w.rearrange("(j p) c -> p j c", p=128)
        pieces = [A.f32(8 * 768).rearrange("p (j c) -> p j c", c=768) for _ in range(2)]
        R_piece = [Res(), Res()]
        for q in range(8):
            pc = pieces[q % 2]
            P.dma("sp", pc, adaw_v[:, :, q * 768:(q + 1) * 768], (), (R_piece[q % 2],))
            for mm_ in range(6):
                m = q * 6 + mm_
                for j in range(8):
                    P.mm(banks[0][:, 2 * m:2 * m + 2], pc[:, j, mm_ * 128:(mm_ + 1) * 128], sc[:, j, :],
                         j == 0, j == 7, (R_piece[q % 2], R_sc), (RB[0],))
        P.tt("dve", mod, banks[0][:, 0:96].rearrange("p (m s) -> p m s", s=2),
             ada_b.unsqueeze(2).broadcast_to([128, 48, 2]), ALU.add, (RB[0], R_vec), (R_mod,))
        P.ts("dve", g1s, mod[:, 8:16, :], 1.0, ALU.add, (R_mod,), (R_mod,))
        P.tt("dve", g1s, g1s, g_mix.unsqueeze(2).broadcast_to([128, 8, 2]), ALU.mult, (R_mod, R_vec), (R_mod,))
        P.ts("dve", g2s, mod[:, 32:40, :], 1.0, ALU.add, (R_mod,), (R_mod,))
        P.tt("dve", g2s, g2s, g_moe.unsqueeze(2).broadcast_to([128, 8, 2]), ALU.mult, (R_mod, R_vec), (R_mod,))
        P.barrier()
        A.release(m0)

        def rstd_from_ss(out_sb, ss_ps, inv_n, R, W, tmp):
            P.act(tmp, ss_ps, AF.Ln, R, W, bias=eps_ap, scale=inv_n)
            P.act(out_sb, tmp, AF.Exp, W, W, scale=-0.5)

        eps_t = A.f32(1)
        eps_ap = eps_t[:, 0:1]
        P.memset("dve", eps_ap, EPS, (R_const,))

        m1 = A.mark()
        R_w1 = Res()
        wib = A.bf(8 * 1952).rearrange("p (j c) -> p j c", c=1952)
        wpsw = A.bf(8 * 32).rearrange("p (j c) -> p j c", c=32)
        wuq = A.bf(2 * 768).rearrange("p (j c) -> p j c", c=768)
        wuqs = A.bf(2 * 768).rearrange("p (j c) -> p j c", c=768)
        mst = A.mark()
        stg = [A.f32(1952) for _ in range(2)]
        R_stg = [Res(), Res()]
        k = 0
        for j in range(8):
            P.dma("sp", stg[k % 2], w_in[j * 128:(j + 1) * 128, :], (), (R_stg[k % 2],))
            P.copy("dve" if k % 2 == 0 else "pool", wib[:, j, :], stg[k % 2], (R_stg[k % 2],), (R_w1,))
            k += 1
        P.dma("sp", stg[k % 2][:, 0:256].rearrange("p (j c) -> p j c", c=32),
              w_pe_sw.rearrange("(j p) c -> p j c", p=128), (), (R_stg[k % 2],))
        P.copy("dve", wpsw, stg[k % 2][:, 0:256].rearrange("p (j c) -> p j c", c=32), (R_stg[k % 2],), (R_w1,))
        k += 1
        for (src, dst) in ((w_uq, wuq), (w_uq_sw, wuqs)):
            P.dma("sp", stg[k % 2][:, 0:1536].rearrange("p (j c) -> p j c", c=768),
                  src.rearrange("(j p) c -> p j c", p=128), (), (R_stg[k % 2],))
            P.copy("dve", dst, stg[k % 2][:, 0:1536].rearrange("p (j c) -> p j c", c=768), (R_stg[k % 2],), (R_w1,))
            k += 1
        C_CQ, C_KV, C_PE, C_QD, C_KD, C_VD = 0, 256, 384, 416, 928, 1440

        xt = [A.f32(8 * 512).rearrange("p (j c) -> p j c", c=512) for _ in range(3)]
        R_x = [Res(), Res(), Res()]
        sq = A.bf(8 * 512).rearrange("p (j c) -> p j c", c=512)
        R_sq = Res()
        h32 = A.f32(4 * 512).rearrange("p (j c) -> p j c", c=512)
        R_h32 = Res()
        hb = [A.bf(8 * 512).rearrange("p (j c) -> p j c", c=512) for _ in range(2)]
        R_hb = [Res(), Res()]
        rstd = A.f32(512)
        lnt = A.f32(512)
        R_rstd = Res()
        rstd2 = A.f32(512)
        lnt2 = A.f32(512)
        R_rstd2 = Res()
        sq2 = A.bf(2 * 512).rearrange("p (j c) -> p j c", c=512)
        R_sq2 = Res()
        ckvn_sb = [A.bf(512) for _ in range(2)]
        R_ckvn_sb = [Res(), Res()]
        cst = [A.f32(512, 32) for _ in range(2)]
        snt = [A.f32(512, 32) for _ in range(2)]
        R_cs = [Res(), Res()]
        t1 = A.f32(512)
        t2 = A.f32(512)
        R_t12 = Res()
        kper = [A.bf(512, 32) for _ in range(2)]
        R_kper = [Res(), Res()]
        ev = [A.bf(4 * 512).rearrange("p (j c) -> p j c", c=512) for _ in range(2)]
        R_ev = [Res() for _ in range(2)]
        vev = [A.bf(4 * 520).rearrange("p (j c) -> p j c", c=520) for _ in range(2)]
        R_vev = [Res(), Res()]
        cqn = A.bf(2 * 512).rearrange("p (j c) -> p j c", c=512)
        R_cqn = Res()
        qev = [A.bf(512) for _ in range(2)]
        R_qev = [Res(), Res()]
        csq = [A.f32(512) for _ in range(2)]
        snq = [A.f32(512) for _ in range(2)]
        R_csq = [Res(), Res()]
        valid = {s: A.f32(48) for s in "ps"}
        R_valid = Res()
        P.dma("sp", valid["p"], valid_in["p"], (), (R_valid,))
        P.dma("sp", valid["s"][:, 0:32], valid_in["s"], (), (R_valid,))
        for b in range(2):
            P.memset("pool", vev[b], 0.0, (R_vev[b],))

        cnt = {"ev": 0, "vev": 0, "qev": 0, "rot": 0}
        ROT = [5, 6, 7]

        def nextbank():
            b = ROT[cnt["rot"] % 3]
            cnt["rot"] += 1
            return b

        tiles = []
        for s in "ps":
            nt_ = SEQ[s] // 512
            nh_ = (NOWN[s] + 2 * HALO) // 512
            A_ = list(range(nh_))
            B_ = list(range(nh_, nt_))
            ia = ib = 0
            while ia < len(A_) or ib < len(B_):
                if ib >= len(B_) or (ia < len(A_) and ia * len(B_) <= ib * len(A_)):
                    tiles.append((s, A_[ia]))
                    ia += 1
                else:
                    tiles.append((s, B_[ib]))
                    ib += 1

        rstd_b = [rstd, A.f32(512)]
        R_h32j = [Res() for _ in range(8)]
        R_rstd_b = [R_rstd, Res()]

        def stageA1(i):
            s, t = tiles[i]
            b = i % 2
            x3 = i % 3
            c0 = t * 512
            P.dma("sp", xt[x3], xT[s].rearrange("(j p) c -> p j c", p=128)[:, :, c0:c0 + 512], (), (R_x[x3],))
            P.act(sq, xt[x3], AF.Square, (R_x[x3],), (R_sq,))
            yield
            for j in range(8):
                P.mm(banks[0], ones_bf, sq[:, j, :], j == 0, j == 7, (R_sq, R_const), (RB[0],))
            yield
            P.act(lnt, banks[0], AF.Ln, (RB[0], R_const), (R_rstd_b[b],), bias=eps_ap, scale=1.0 / D)
            yield
            P.act(rstd_b[b], lnt, AF.Exp, (R_rstd_b[b],), (R_rstd_b[b],), scale=-0.5)
            yield

        def stageB1(i):
            s, t = tiles[i]
            si = SIDX[s]
            b = i % 2
            x3 = i % 3
            for j in range(8):
                j4 = j % 4
                P.tt("dve", h32[:, j4, :], xt[x3][:, j, :], rstd_b[b], ALU.mult, (R_x[x3], R_rstd_b[b]), (R_h32j[j4],))
                if j % 2 == 0:
                    P.act(hb[b][:, j, :], h32[:, j4, :], AF.Identity, (R_h32j[j4], R_mod), (R_hb[b],),
                          bias=mod[:, j, si:si + 1], scale=g1s[:, j, si:si + 1])
                else:
                    P.ts("dve", hb[b][:, j, :], h32[:, j4, :], g1s[:, j, si:si + 1], ALU.mult, (R_h32j[j4], R_mod), (R_hb[b],),
                         s2=mod[:, j, si:si + 1], op1=ALU.add)
                yield

        def proj(bank, wcols, b, M=128, w=None):
            w = wib if w is None else w
            for j in range(8):
                P.mm(banks[bank][0:M, :], w[:, j, wcols:wcols + M], hb[b][:, j, :], j == 0, j == 7,
                     (R_hb[b], R_w1), (RB[bank],))

        def main(i):
            s, t = tiles[i]
            si = SIDX[s]
            b = i % 2
            c0 = t * 512
            n = NOWN[s]
            proj(1, C_KV, b)
            yield
            proj(2, C_PE, b, M=32)
            proj(3, 0, b, M=32, w=wpsw)
            yield
            P.dma("sp", cst[b], cs_in[s][:, c0:c0 + 512], (), (R_cs[b],))
            P.dma("sp", snt[b], sn_in[s][:, c0:c0 + 512], (), (R_cs[b],))
            P.act(sq2[:, 0, :], banks[1], AF.Square, (RB[1],), (R_sq2,))
            P.mm(banks[4], ones_bf, sq2[:, 0, :], True, True, (R_sq2, R_const), (RB[4],))
            rstd_from_ss(rstd2, banks[4], 1.0 / 128, (RB[4], R_const), (R_rstd2,), lnt2)
            yield
            P.stt("dve", ckvn_sb[b], banks[1], kvg, rstd2, ALU.mult, ALU.mult, (RB[1], R_rstd2, R_vec), (R_ckvn_sb[b],))
            P.dma("pool", ckvn_d[s][:, c0:c0 + 512], ckvn_sb[b], (R_ckvn_sb[b],), (R_ckvn[s],))
            yield
            P.tt("dve", t1[0:32, :], banks[2][0:32, :], cst[b], ALU.mult, (RB[2], R_cs[b]), (R_t12,))
            P.tt("dve", t2[0:32, :], banks[3][0:32, :], snt[b], ALU.mult, (RB[3], R_cs[b]), (R_t12,))
            P.tt("dve", kper[b], t1[0:32, :], t2[0:32, :], ALU.add, (R_t12,), (R_kper[b],))
            P.dma("pool", kpe_d[s][:, c0:c0 + 512], kper[b], (R_kper[b],), (R_kpe[s],))
            yield
            if c0 < n + 2 * HALO:
                e = cnt["ev"] % 2
                cnt["ev"] += 1
                for c in range(4):
                    bk = nextbank()
                    proj(bk, C_KD + c * 128, b)
                    P.copy("act" if c % 2 == 0 else "dve", ev[e][:, c, :], banks[bk], (RB[bk],), (R_ev[e],))
                    yield
                P.dma("pool", kd_d[s].rearrange("(c p) n -> p c n", p=128)[:, :, c0:c0 + 512], ev[e], (R_ev[e],), (R_kd[s],))
                v = cnt["vev"] % 2
                cnt["vev"] += 1
                for tc in range(4):
                    bk = nextbank()
                    for j in range(8):
                        P.mm(banks[bk], hb[b][:, j, tc * 128:(tc + 1) * 128], wib[:, j, C_VD:C_VD + 512], j == 0, j == 7,
                             (R_hb[b], R_w1), (RB[bk],))
                    vcol = valid[s][:, t * 4 + tc:t * 4 + tc + 1]
                    P.ts("dve", vev[v][:, tc, :].rearrange("p (h c) -> p h c", c=65)[:, :, 0:64],
                         banks[bk].rearrange("p (h c) -> p h c", c=64), vcol, ALU.mult, (RB[bk], R_valid), (R_vev[v],))
                    P.copy("pool", vev[v][:, tc, :].rearrange("p (h c) -> p h c", c=65)[:, :, 64:65],
                           vcol.unsqueeze(1).broadcast_to([128, 8, 1]), (R_valid,), (R_vev[v],))
                    yield
                P.dma("pool", vd_d[s][c0:c0 + 512, :].rearrange("(c p) f -> p c f", p=128), vev[v], (R_vev[v],), (R_vd[s],))
            if HALO <= c0 < HALO + n:
                q0 = c0 - HALO
                e = cnt["ev"] % 2
                cnt["ev"] += 1
                for c in range(4):
                    bk = nextbank()
                    proj(bk, C_QD + c * 128, b)
                    P.copy("act" if c % 2 == 0 else "dve", ev[e][:, c, :], banks[bk], (RB[bk],), (R_ev[e],))
                    yield
                P.dma("pool", qd_d[s].rearrange("(c p) n -> p c n", p=128)[:, :, q0:q0 + 512], ev[e], (R_ev[e],), (R_qd[s],))
                bks = [nextbank(), nextbank()]
                for c in range(2):
                    proj(bks[c], C_CQ + c * 128, b)
                    P.act(sq2[:, c, :], banks[bks[c]], AF.Square, (RB[bks[c]],), (R_sq2,))
                for c in range(2):
                    P.mm(banks[4], ones_bf, sq2[:, c, :], c == 0, c == 1, (R_sq2, R_const), (RB[4],))
                rstd_from_ss(rstd2, banks[4], 1.0 / 256, (RB[4], R_const), (R_rstd2,), lnt2)
                yield
                for c in range(2):
                    P.stt("dve", cqn[:, c, :], banks[bks[c]], qg[:, c:c + 1], rstd2, ALU.mult, ALU.mult,
                          (RB[bks[c]], R_rstd2, R_vec), (R_cqn,))
                P.dma("sp", csq[b][64:96, :], cs_in[s][:, c0:c0 + 512], (), (R_csq[b],))
                P.dma("sp", snq[b][64:96, :], sn_in[s][:, c0:c0 + 512], (), (R_csq[b],))
                for h in range(8):
                    ba = nextbank()
                    bb = nextbank()
                    for c in range(2):
                        P.mm(banks[ba][0:96, :], wuq[:, c, h * 96:(h + 1) * 96], cqn[:, c, :], c == 0, c == 1,
                             (R_cqn, R_w1), (RB[ba],))
                    for c in range(2):
                        P.mm(banks[bb][0:96, :], wuqs[:, c, h * 96:(h + 1) * 96], cqn[:, c, :], c == 0, c == 1,
                             (R_cqn, R_w1), (RB[bb],))
                    qe = cnt["qev"] % 2
                    cnt["qev"] += 1
                    P.copy("act", qev[qe][0:64, :], banks[ba][0:64, :], (RB[ba],), (R_qev[qe],))
                    P.tt("dve", t1[64:96, :], banks[ba][64:96, :], csq[b][64:96, :], ALU.mult, (RB[ba], R_csq[b]), (R_t12,))
                    P.tt("dve", t2[64:96, :], banks[bb][64:96, :], snq[b][64:96, :], ALU.mult, (RB[bb], R_csq[b]), (R_t12,))
                    P.tt("dve", qev[qe][64:96, :], t1[64:96, :], t2[64:96, :], ALU.add, (R_t12,), (R_qev[qe],))
                    P.dma("pool", qm_d[s][h, :, q0:q0 + 512], qev[qe][0:96, :], (R_qev[qe],), (R_qm[s],))
                    yield

        NT = len(tiles)

        def _empty():
            return
            yield

        for i in range(NT + 2):
            gens = [main(i - 2) if 2 <= i else _empty(),
                    stageB1(i - 1) if 1 <= i <= NT else _empty(),
                    stageA1(i) if i < NT else _empty()]
            done = [False, False, False]
            while not all(done):
                for gi in range(3):
                    if not done[gi]:
                        try:
                            next(gens[gi])
                        except StopIteration:
                            done[gi] = True
        P.barrier()
        A.release(m1)

        def finalize(o_sb, ncols, out_bf, bank, R_o, R_out, rden, R_rden):
            P.recip(rden[64:65, 0:ncols], o_sb[64:65, 0:ncols], (R_o,), (R_rden,))
            P.mm(banks[bank][0:64, 0:ncols], ones32[64:65, 0:64], rden[64:65, 0:ncols], True, True,
                 (R_rden, R_const), (RB[bank],))
            P.tt("dve", out_bf, o_sb[0:64, 0:ncols], banks[bank][0:64, 0:ncols], ALU.mult, (R_o, RB[bank]), (R_out,))

        m3 = A.mark()
        wkv_st = A.f32(1024)
        wkv = A.bf(1024)
        R_wkv = Res()
        P.dma("sp", wkv_st, w_ukv, (), (R_wkv,))
        P.copy("dve", wkv, wkv_st, (R_wkv,), (R_wkv,))
        ckvn = A.bf(SP)
        R_ck = Res()
        Kt = [A.bf(SP) for _ in range(2)]
        R_K = [Res(), Res()]
        Vt = [A.bf(128 * 65).rearrange("p (c f) -> p c f", f=65) for _ in range(2)]
        R_V = [Res(), Res()]
        Qh = [A.bf(NP_) for _ in range(2)]
        R_Q = [Res(), Res()]
        pT = [A.bf(1536) for _ in range(3)]
        R_pT = [Res() for _ in range(3)]
        o_sb = [A.f32(512) for _ in range(2)]
        R_osb = [Res(), Res()]
        rden = A.f32(512)
        R_rden = Res()
        ybf = [A.bf(512) for _ in range(2)]
        R_ybf = [Res(), Res()]
        for b in range(2):
            P.memset("pool", Vt[b][:, :, 64:65], 1.0, (R_V[b],))
        SCALE_A = 96.0 ** -0.5
        O_B = [6, 6]
        X_B = [7, 7]
        gcnt = {"st": 0, "o": 0, "x": 0, "y": 0, "pt": 0}
        for s in "ps":
            S_ = SEQ[s]
            n = NOWN[s]
            nkc = S_ // 128
            P.dma("sp", ckvn[:, 0:S_], ckvn_d[s], (R_ckvn[s],), (R_ck,))
            for b in range(2):
                P.dma("sp", Kt[b][64:96, 0:S_], kpe_d[s], (R_kpe[s],), (R_K[b],))
            def expand(h):
                kb = h % 2
                for t in range(S_ // 512):
                    xb = X_B[gcnt["x"] % 2]
                    gcnt["x"] += 1
                    P.mm(banks[xb][0:64, :], wkv[:, h * 128:h * 128 + 64], ckvn[:, t * 512:(t + 1) * 512], True, True,
                         (R_wkv, R_ck), (RB[xb],))
                    P.copy("dve", Kt[kb][0:64, t * 512:(t + 1) * 512], banks[xb][0:64, :], (RB[xb],), (R_K[kb],))
                    yield
                for t in range(S_ // 1024):
                    xb = X_B[gcnt["x"] % 2]
                    gcnt["x"] += 1
                    for c in range(8):
                        kc = t * 8 + c
                        P.mm(banks[xb][:, c * 64:(c + 1) * 64], ckvn[:, kc * 128:(kc + 1) * 128],
                             wkv[:, h * 128 + 64:h * 128 + 128], True, True, (R_wkv, R_ck), (RB[xb],))
                    P.copy("dve", Vt[kb][:, t * 8:(t + 1) * 8, 0:64], banks[xb].rearrange("p (c f) -> p c f", f=64),
                           (RB[xb],), (R_V[kb],))
                    yield

            for _ in expand(0):
                pass
            for h in range(8):
                kb = h % 2
                gen_next = expand(h + 1) if h + 1 < 8 else None
                nsteps = (S_ // 512 + S_ // 1024)
                tot_iters = (n // 512) * ((nkc + 2) // 3)
                every = max(1, tot_iters // (nsteps + 1))
                itc = 0
                P.dma("sp", Qh[kb][0:96, 0:n], qm_d[s][h], (R_qm[s],), (R_Q[kb],))
                for qt in range(n // 512):
                    ob = O_B[gcnt["o"] % 2]
                    gcnt["o"] += 1
                    qs = Qh[kb][0:96, qt * 512:(qt + 1) * 512]

                    groups = []
                    kc_ = 0
                    while kc_ < nkc:
                        g_ = min(3, nkc - kc_)
                        groups.append((kc_, g_))
                        kc_ += g_
                    ngr = len(groups)

                    def S_grp(gi):
                        k0, g_ = groups[gi]
                        sp_ = gcnt["st"] % 2
                        gcnt["st"] += 1
                        for u in range(g_):
                            kc = k0 + u
                            P.mm(banks[3 * sp_ + u], Kt[kb][0:96, kc * 128:(kc + 1) * 128], qs, True, True,
                                 (R_K[kb], R_Q[kb]), (RB[3 * sp_ + u],))
                        return sp_
                    sps = {0: S_grp(0), 1: S_grp(1)}
                    for k in range(ngr):
                        itc += 1
                        if gen_next is not None and itc % every == 0:
                            try:
                                next(gen_next)
                            except StopIteration:
                                gen_next = None
                        k0, g_ = groups[k]
                        sp_ = sps.pop(k)
                        pb = gcnt["pt"] % 3
                        gcnt["pt"] += 1
                        P.act(pT[pb][:, 0:g_ * 512], tri[sp_][:, 0:g_ * 512], AF.Exp,
                              tuple(RB[3 * sp_ + u] for u in range(g_)), (R_pT[pb],), scale=SCALE_A)
                        if k + 2 < ngr:
                            sps[k + 2] = S_grp(k + 2)
                        for u in range(g_):
                            kc = k0 + u
                            P.mm(banks[ob][0:65, :], Vt[kb][:, kc, :], pT[pb][:, u * 512:(u + 1) * 512], kc == 0, kc == nkc - 1,
                                 (R_V[kb], R_pT[pb]), (RB[ob],))
                    yb_ = gcnt["y"] % 2
                    gcnt["y"] += 1
                    P.copy("dve", o_sb[yb_][0:65, :], banks[ob][0:65, :], (RB[ob],), (R_osb[yb_],))
                    finalize(o_sb[yb_], 512, ybf[yb_][0:64, :], 7, R_osb[yb_], R_ybf[yb_], rden, R_rden)
                    P.dma("pool", ya_d[s][h * 64:(h + 1) * 64, qt * 512:(qt + 1) * 512], ybf[yb_][0:64, :],
                          (R_ybf[yb_],), (R_ya[s],))
                if gen_next is not None:
                    for _ in gen_next:
                        pass
        P.barrier()
        A.release(m3)

        m4 = A.mark()
        dmask32 = A.f32(48 * 128).rearrange("p (m f) -> p m f", f=128)
        dmask = A.bf(48 * 128).rearrange("p (m f) -> p m f", f=128)
        R_dm = Res()
        P.dma("sp", dmask32, dmask_in.rearrange("p (m f) -> p m f", f=128), (), (R_dm,))
        P.copy("dve", dmask, dmask32, (R_dm,), (R_dm,))
        NBLK = {s: [NOWN[s] // 128 + d for d in DPAT] for s in "ps"}
        NBT = sum(NBLK["p"])
        Vd = [A.bf(NBT * 130).rearrange("p (c f) -> p c f", f=130) for _ in range(2)]
        R_Vd = [Res(), Res()]
        qTd = [A.bf(NP_) for _ in range(2)]
        kTd = [A.bf(NP_ + 2 * HALO) for _ in range(2)]
        R_qk = [Res(), Res()]
        accd = [A.f32(NP_) for _ in range(2)]
        R_acc = [Res(), Res()]
        ebf = [A.bf(512) for _ in range(4)]
        R_ebf = [Res() for _ in range(4)]
        pTd = [A.bf(512) for _ in range(4)]
        R_pTd = [Res() for _ in range(4)]
        lnb = A.f32(512)
        rb4 = A.f32(512)
        R_fin4 = Res()
        ybf4 = [A.bf(512) for _ in range(2)]
        R_ybf4 = [Res(), Res()]
        SA_B = [0, 1, 2, 3]
        OD_B = [4, 5]
        c4 = {"o": 0, "y": 0, "hp": 0, "h": 0, "g": 0}
        items = []
        for s in "ps":
            n = NOWN[s]
            for hp in range(4):
                for hh in range(2):
                    for di, d in enumerate(DPAT):
                        pairs = []
                        if d == 1:
                            for qt in range(n // 128):
                                pairs.append((0, qt))
                        else:
                            for qt in range(n // (128 * d)):
                                for c in range(d):
                                    pairs.append((c, qt))
                        ng = len(pairs) // 4
                        for gi in range(ng):
                            items.append(dict(s=s, hp=hp, hh=hh, di=di, d=d, grp=pairs[gi * 4:gi * 4 + 4],
                                              first_pair=(hh == 0 and di == 0 and gi == 0),
                                              first_head=(di == 0 and gi == 0),
                                              last_head=(di == 2 and gi == ng - 1)))
        state = {}

        def stageA(it):
            s = it["s"]
            n = NOWN[s]
            d = it["d"]
            di = it["di"]
            hp = it["hp"]
            hh = it["hh"]
            h = hp * 2 + hh
            if it["first_pair"]:
                vb = c4["hp"] % 2
                c4["hp"] += 1
                state["vb"] = vb
                boff = []
                o_ = 0
                for d2 in DPAT:
                    boff.append(o_)
                    nb2 = n // (128 * d2) + 1
                    for c in range(d2):
                        r0 = c + HALO - 64 * d2
                        src = vd_d[s][r0:r0 + d2 * (128 * nb2 - 1) + 1:d2, hp * 130:(hp + 1) * 130]
                        src = src.rearrange("(k p) f -> p k f", p=128)
                        P.dma("sp", Vd[vb][:, o_ + c * nb2:o_ + (c + 1) * nb2, :], src, (R_vd[s],), (R_Vd[vb],))
                    o_ += nb2 * d2
                state["boff"] = boff
            if it["first_head"]:
                hb_ = c4["h"] % 2
                c4["h"] += 1
                state["hb"] = hb_
                P.dma("sp", qTd[hb_][0:64, 0:n], qd_d[s][h * 64:(h + 1) * 64, :], (R_qd[s],), (R_qk[hb_],))
                P.dma("sp", kTd[hb_][0:64, 0:n + 2 * HALO], kd_d[s][h * 64:(h + 1) * 64, :], (R_kd[s],), (R_qk[hb_],))
            hb_ = state["hb"]
            vb = state["vb"]
            g = c4["g"] % 2
            c4["g"] += 1
            sA, sBk = SA_B[2 * g], SA_B[2 * g + 1]
            for qi, (c, qt) in enumerate(it["grp"]):
                i0 = HALO // d + 128 * qt
                rq = c + d * i0 - HALO
                rhs = qTd[hb_][0:64, rq:rq + 127 * d + 1:d] if d > 1 else qTd[hb_][0:64, rq:rq + 128]
                for (bkk, ioff) in ((sA, i0 - 64), (sBk, i0 + 64)):
                    rk = c + d * ioff
                    lhs = kTd[hb_][0:64, rk:rk + 127 * d + 1:d] if d > 1 else kTd[hb_][0:64, rk:rk + 128]
                    P.mm(banks[bkk][:, qi * 128:(qi + 1) * 128], lhs, rhs, True, True, (R_qk[hb_],), (RB[bkk],))
            pts = []
            for ab, bkk in enumerate((sA, sBk)):
                eb = 2 * g + ab
                P.act(ebf[eb], banks[bkk], AF.Exp, (RB[bkk],), (R_ebf[eb],), scale=0.125)
                mk = dmask[:, (h * 3 + di) * 2 + ab, :].unsqueeze(1).broadcast_to([128, 4, 128])
                P.tt("dve", pTd[eb].rearrange("p (q f) -> p q f", f=128),
                     ebf[eb].rearrange("p (q f) -> p q f", f=128), mk, ALU.mult, (R_ebf[eb], R_dm), (R_pTd[eb],))
                pts.append(eb)
            return dict(hb=hb_, vb=vb, pts=pts, boff=state["boff"])

        def stageB(it, st):
            s = it["s"]
            n = NOWN[s]
            d = it["d"]
            di = it["di"]
            hh = it["hh"]
            h = it["hp"] * 2 + hh
            hb_, vb, pts, boff = st["hb"], st["vb"], st["pts"], st["boff"]
            nb = n // (128 * d) + 1
            acc = accd[hb_]
            ob = OD_B[c4["o"] % 2]
            c4["o"] += 1
            grp = it["grp"]
            for qi, (c, qt) in enumerate(grp):
                blkA = boff[di] + c * nb + qt
                for ab in range(2):
                    P.mm(banks[ob][0:65, qi * 128:(qi + 1) * 128], Vd[vb][:, blkA + ab, hh * 65:(hh + 1) * 65],
                         pTd[pts[ab]][:, qi * 128:(qi + 1) * 128], ab == 0, ab == 1,
                         (R_Vd[vb], R_pTd[pts[ab]]), (RB[ob],))
            c_0, qt0 = grp[0]
            if d == 1:
                av = acc[0:65, qt0 * 128:qt0 * 128 + 512]
                pv = banks[ob][0:65, :]
            else:
                base = 128 * d * qt0
                av = acc[0:65, base:base + 128 * d].rearrange("p (f c) -> p c f", c=d)[:, c_0:c_0 + 4, :]
                pv = banks[ob][0:65, :].rearrange("p (c f) -> p c f", f=128)
            if di == 0:
                P.copy("dve", av, pv, (RB[ob],), (R_acc[hb_],))
            else:
                P.tt("dve", av, av, pv, ALU.add, (RB[ob], R_acc[hb_]), (R_acc[hb_],))
            if it["last_head"]:
                for qt in range(n // 512):
                    yb_ = c4["y"] % 2
                    c4["y"] += 1
                    cs_ = slice(qt * 512, (qt + 1) * 512)
                    P.mm(banks[6][0:64, :], ones32[64:65, 0:64], acc[64:65, cs_], True, True, (R_acc[hb_], R_const), (RB[6],))
                    P.act(lnb[0:64, :], banks[6][0:64, :], AF.Ln, (RB[6],), (R_fin4,))
                    P.act(rb4[0:64, :], lnb[0:64, :], AF.Exp, (R_fin4,), (R_fin4,), scale=-1.0)
                    P.tt("dve", ybf4[yb_][0:64, :], acc[0:64, cs_], rb4[0:64, :], ALU.mult, (R_acc[hb_], R_fin4), (R_ybf4[yb_],))
                    P.dma("pool", yb_d[s][h * 64:(h + 1) * 64, qt * 512:(qt + 1) * 512], ybf4[yb_][0:64, :],
                          (R_ybf4[yb_],), (R_yb[s],))

        sts = {0: stageA(items[0])}
        for i in range(len(items)):
            if i + 1 < len(items):
                sts[i + 1] = stageA(items[i + 1])
            stageB(items[i], sts.pop(i))
        P.barrier()
        A.release(m4)

        m5 = A.mark()
        TB = 1024
        R_w5 = Res()
        wob = A.bf(8 * 1024).rearrange("p (c n) -> p c n", n=1024)
        wr32 = A.f32(8 * 20).rearrange("p (j c) -> p j c", c=20)
        selb = A.bf(NEXP * 128, 16)
        NSTG = 4
        wstg = [A.f32(4 * 512) for _ in range(NSTG)]
        R_wstg = [Res() for _ in range(NSTG)]
        sc5 = {"stg": 0}

        def load_cast(dst_bf, src_ap, shape_c, eng):
            k_ = sc5["stg"] % NSTG
            sc5["stg"] += 1
            jj, cc = shape_c
            st = wstg[k_][:, 0:jj * cc].rearrange("p (j c) -> p j c", c=cc)
            P.dma("sp", st, src_ap, (), (R_wstg[k_],))
            return k_, st

        for half in range(4):
            k_, st = load_cast(None, w_out.rearrange("(c p) n -> p c n", p=128)[:, half * 2:(half + 1) * 2, :], (2, 1024), "dve")
            P.copy("dve", wob[:, half * 2:(half + 1) * 2, :], st, (R_wstg[k_],), (R_w5,))
        P.dma("sp", wr32, w_r.rearrange("(j p) c -> p j c", p=128), (), (R_w5,))
        k_ = sc5["stg"] % NSTG
        sc5["stg"] += 1
        P.dma("sp", wstg[k_][0:16, 0:NEXP * 128], sel_in, (), (R_wstg[k_],))
        P.copy("dve", selb, wstg[k_][0:16, 0:NEXP * 128], (R_wstg[k_],), (R_w5,))

        acc5 = A.f32(8 * TB).rearrange("p (j c) -> p j c", c=TB)
        R_acc5 = Res()
        h2b = A.bf(8 * TB).rearrange("p (j c) -> p j c", c=TB)
        R_h2b = Res()
        h2f = A.f32(8 * 512).rearrange("p (j c) -> p j c", c=512)
        R_h2f = Res()
        yab = A.bf(8 * 512).rearrange("p (c n) -> p c n", n=512)
        R_yab = Res()
        sq5 = yab
        R_sq5 = R_yab
        rstd5 = A.f32(512)
        lnt5 = A.f32(512)
        R_rstd5 = Res()
        wcT = A.bf(TB, 16)
        R_wcT = Res()
        wg = [A.bf(8 * 512).rearrange("p (j c) -> p j c", c=512) for _ in range(2)]
        wu = [A.bf(8 * 512).rearrange("p (j c) -> p j c", c=512) for _ in range(2)]
        wd = [A.bf(4 * 1024).rearrange("p (j c) -> p j c", c=1024) for _ in range(2)]
        R_wg = [Res(), Res()]
        R_wu = [Res(), Res()]
        R_wd = [Res(), Res()]
        wcb = [A.bf(512) for _ in range(2)]
        R_wcb = [Res(), Res()]
        sa = [A.bf(512) for _ in range(2)]
        R_sa = [Res(), Res()]
        uw = [A.bf(512) for _ in range(2)]
        R_uw = [Res(), Res()]
        hid = [A.bf(4 * 512).rearrange("p (j c) -> p j c", c=512) for _ in range(2)]
        R_hid = [Res(), Res()]
        rl = A.f32(80).rearrange("p (q c) -> p q c", c=20)
        r_a = A.f32(16).rearrange("p (q c) -> p q c", c=4)
        goh = A.f32(16).rearrange("p (q c) -> p q c", c=4)
        r_m = A.f32(4)
        r_s = A.f32(4)
        gw = A.f32(4)
        esel = A.f32(64).rearrange("p (q g e) -> p q g e", g=4, e=4)
        el = A.f32(16).rearrange("p (q c) -> p q c", c=4)
        ee = A.f32(16).rearrange("p (q c) -> p q c", c=4)
        mk1 = A.f32(16).rearrange("p (q c) -> p q c", c=4)
        ee2 = A.f32(16).rearrange("p (q c) -> p q c", c=4)
        mk2 = A.f32(16).rearrange("p (q c) -> p q c", c=4)
        m1_ = A.f32(4)
        m2_ = A.f32(4)
        wexp = A.f32(16).rearrange("p (q c) -> p q c", c=4)
        wc = A.f32(64).rearrange("p (q g e) -> p q g e", g=4, e=4)
        R_rt = Res()
        yout = [A.f32(512) for _ in range(2)]
        R_yout = [Res(), Res()]

        def bc4(ap):
            return ap.unsqueeze(2).broadcast_to([128, 4, 4])

        c5 = {"pb": 0, "e": 0, "ob": 0, "y": 0, "w": 0}
        PB5 = [0, 1, 2, 3]
        OB5 = [4, 5]
        blocks = []
        for s in "ps":
            for bi in range(NOWN[s] // TB):
                blocks.append((s, bi))
        PIECES = []
        for gb in range(len(blocks)):
            for e in range(NEXP):
                wb_ = (gb * NEXP + e) % 2
                for hf in range(2):
                    PIECES.append((w_gate[e].rearrange("(j p) c -> p j c", p=128)[:, hf * 4:(hf + 1) * 4, :],
                                   wg[wb_][:, hf * 4:(hf + 1) * 4, :], R_wg[wb_], 512))
                for hf in range(2):
                    PIECES.append((w_up[e].rearrange("(j p) c -> p j c", p=128)[:, hf * 4:(hf + 1) * 4, :],
                                   wu[wb_][:, hf * 4:(hf + 1) * 4, :], R_wu[wb_], 512))
                for hf in range(2):
                    PIECES.append((w_down[e].rearrange("(j p) c -> p j c", p=128)[:, hf * 2:(hf + 1) * 2, :],
                                   wd[wb_][:, hf * 2:(hf + 1) * 2, :], R_wd[wb_], 1024))
        pc = {"dma": 0, "cast": 0}
        pstg = {}

        def piece_dma():
            i = pc["dma"]
            if i >= len(PIECES):
                return
            pc["dma"] += 1
            src, dst, Rd, cc = PIECES[i]
            k_ = sc5["stg"] % NSTG
            sc5["stg"] += 1
            st = wstg[k_].rearrange("p (j c) -> p j c", c=cc)
            P.dma("sp", st, src, (), (R_wstg[k_],))
            pstg[i] = (k_, st)

        def piece_cast():
            i = pc["cast"]
            if i >= len(PIECES):
                return
            pc["cast"] += 1
            src, dst, Rd, cc = PIECES[i]
            k_, st = pstg.pop(i)
            P.copy("act", dst, st, (R_wstg[k_],), (Rd,))

        def slot():
            piece_cast()
            piece_dma()

        for _ in range(3):
            piece_dma()
        for _ in range(6):
            slot()
        ucnt = {"u": 0}
        for gb, (s, bi) in enumerate(blocks):
            si = SIDX[s]
            for tl in range(TB // 512):
                q0 = bi * TB + tl * 512
                tc0 = tl * 512
                P.dma("sp", acc5[:, :, tc0:tc0 + 512],
                      xT[s].rearrange("(j p) c -> p j c", p=128)[:, :, HALO + q0:HALO + q0 + 512], (), (R_acc5,))
                P.dma("sp", yab[:, 0:4, :], ya_d[s].rearrange("(c p) n -> p c n", p=128)[:, :, q0:q0 + 512], (R_ya[s],), (R_yab,))
                P.dma("sp", yab[:, 4:8, :], yb_d[s].rearrange("(c p) n -> p c n", p=128)[:, :, q0:q0 + 512], (R_yb[s],), (R_yab,))
                for j in range(8):
                    bk = PB5[c5["pb"] % 4]
                    c5["pb"] += 1
                    for c in range(8):
                        P.mm(banks[bk], wob[:, c, j * 128:(j + 1) * 128], yab[:, c, :], c == 0, c == 7, (R_w5, R_yab), (RB[bk],))
                    P.stt("dve", acc5[:, j, tc0:tc0 + 512], banks[bk], mod[:, 16 + j, si:si + 1], acc5[:, j, tc0:tc0 + 512],
                          ALU.mult, ALU.add, (RB[bk], R_acc5, R_mod), (R_acc5,))
                P.act(sq5, acc5[:, :, tc0:tc0 + 512], AF.Square, (R_acc5,), (R_sq5,))
                for j in range(8):
                    P.mm(banks[6], ones_bf, sq5[:, j, :], j == 0, j == 7, (R_sq5, R_const), (RB[6],))
                rstd_from_ss(rstd5, banks[6], 1.0 / D, (RB[6], R_const), (R_rstd5,), lnt5)
                for j in range(8):
                    P.stt("dve", h2f[:, j, :], acc5[:, j, tc0:tc0 + 512], g2s[:, j, si:si + 1], rstd5, ALU.mult, ALU.mult,
                          (R_acc5, R_rstd5, R_mod), (R_h2f,))
                for j in range(8):
                    P.act(h2f[:, j, :], h2f[:, j, :], AF.Identity, (R_h2f, R_mod), (R_h2f,), bias=mod[:, 24 + j, si:si + 1])
                for j in range(8):
                    P.copy("dve", h2b[:, j, tc0:tc0 + 512], h2f[:, j, :], (R_h2f,), (R_h2b,))
                for q in range(4):
                    for j in range(8):
                        P.mm(banks[7][:, q * 20:(q + 1) * 20], h2f[:, j, q * 128:(q + 1) * 128], wr32[:, j, :], j == 0, j == 7,
                             (R_h2f, R_w5), (RB[7],))
                P.copy("dve", rl, banks[7][:, 0:80].rearrange("p (q c) -> p q c", c=20), (RB[7],), (R_rt,))
                RT = (R_rt,)
                gl = rl[:, :, 0:4]
                P.reduce(r_m, gl, ALU.max, RT, RT)
                P.tt("dve", r_a, gl, bc4(r_m), ALU.subtract, RT, RT)
                P.tt("dve", goh, gl, bc4(r_m), ALU.is_equal, RT, RT)
                P.act(r_a, r_a, AF.Exp, RT, RT)
                P.reduce(r_s, r_a, ALU.add, RT, RT)
                P.recip(gw, r_s, RT, RT)
                P.tt("dve", esel, rl[:, :, 4:20].rearrange("p q (g e) -> p q g e", e=4),
                     goh.unsqueeze(3).broadcast_to([128, 4, 4, 4]), ALU.mult, RT, RT)
                P.reduce(el, esel.rearrange("p q g e -> p q e g"), ALU.add, RT, RT)
                P.reduce(r_m, el, ALU.max, RT, RT)
                P.tt("dve", ee, el, bc4(r_m), ALU.subtract, RT, RT)
                P.act(ee, ee, AF.Exp, RT, RT)
                P.reduce(m1_, ee, ALU.max, RT, RT)
                P.tt("dve", mk1, ee, bc4(m1_), ALU.is_equal, RT, RT)
                P.tt("dve", ee2, ee, mk1, ALU.mult, RT, RT)
                P.tt("dve", ee2, ee, ee2, ALU.subtract, RT, RT)
                P.reduce(m2_, ee2, ALU.max, RT, RT)
                P.tt("dve", mk2, ee2, bc4(m2_), ALU.is_equal, RT, RT)
                P.tt("dve", mk1, mk1, mk2, ALU.add, RT, RT)
                P.tt("dve", m1_, m1_, m2_, ALU.add, RT, RT)
                P.recip(m1_, m1_, RT, RT)
                P.tt("dve", m1_, m1_, gw, ALU.mult, RT, RT)
                P.tt("dve", wexp, ee, mk1, ALU.mult, RT, RT)
                P.tt("dve", wexp, wexp, bc4(m1_), ALU.mult, RT, RT)
                P.tt("dve", wc, goh.unsqueeze(3).broadcast_to([128, 4, 4, 4]),
                     wexp.unsqueeze(2).broadcast_to([128, 4, 4, 4]), ALU.mult, RT, RT)
                for q in range(4):
                    P.tr(banks[6][0:16, q * 128:(q + 1) * 128],
                         wc[:, q].rearrange("p g e -> p (g e)"), ident, (R_rt, R_const), (RB[6],))
                P.copy("dve", wcT[0:16, tc0:tc0 + 512], banks[6][0:16, :], (RB[6],), (R_wcT,))
            units = []
            for e in range(NEXP):
                for tl in range(TB // 512):
                    units.append((e, tl, (gb * NEXP + e) % 2))

            def GU(u):
                e, tl, wbuf = u
                tc0 = tl * 512
                cb = ucnt["u"] % 2
                ucnt["u"] += 1
                P.mm(banks[7], selb[0:16, e * 128:(e + 1) * 128], wcT[0:16, tc0:tc0 + 512], True, True,
                     (R_w5, R_wcT), (RB[7],))
                P.copy("act", wcb[cb], banks[7], (RB[7],), (R_wcb[cb],))
                for fc in range(4):
                    ba = PB5[c5["pb"] % 4]
                    bu = PB5[(c5["pb"] + 1) % 4]
                    c5["pb"] += 2
                    for j in range(8):
                        P.mm(banks[ba], wg[wbuf][:, j, fc * 128:(fc + 1) * 128], h2b[:, j, tc0:tc0 + 512], j == 0, j == 7,
                             (R_wg[wbuf], R_h2b), (RB[ba],))
                    for j in range(8):
                        P.mm(banks[bu], wu[wbuf][:, j, fc * 128:(fc + 1) * 128], h2b[:, j, tc0:tc0 + 512], j == 0, j == 7,
                             (R_wu[wbuf], R_h2b), (RB[bu],))
                    sb_ = fc % 2
                    P.act(sa[sb_], banks[ba], AF.Silu, (RB[ba],), (R_sa[sb_],))
                    P.tt("dve", uw[sb_], banks[bu], wcb[cb], ALU.mult, (RB[bu], R_wcb[cb]), (R_uw[sb_],))
                    P.tt("dve", hid[cb][:, fc, :], sa[sb_], uw[sb_], ALU.mult, (R_sa[sb_], R_uw[sb_]), (R_hid[cb],))
                    if fc < 3:
                        slot()
                return cb

            def DN(u, cb):
                e, tl, wbuf = u
                tc0 = tl * 512
                for j in range(8):
                    ob = OB5[c5["ob"] % 2]
                    c5["ob"] += 1
                    for fc in range(4):
                        P.mm(banks[ob], wd[wbuf][:, fc, j * 128:(j + 1) * 128], hid[cb][:, fc, :], fc == 0, fc == 3,
                             (R_wd[wbuf], R_hid[cb]), (RB[ob],))
                    P.stt("dve", acc5[:, j, tc0:tc0 + 512], banks[ob], mod[:, 40 + j, si:si + 1], acc5[:, j, tc0:tc0 + 512],
                          ALU.mult, ALU.add, (RB[ob], R_acc5, R_mod), (R_acc5,))

            cbs = {0: GU(units[0])}
            for i in range(len(units)):
                if i + 1 < len(units):
                    cbs[i + 1] = GU(units[i + 1])
                DN(units[i], cbs.pop(i))
            for tl in range(TB // 512):
                tc0 = tl * 512
                q0 = bi * TB + tc0
                P.act(sq5, acc5[:, :, tc0:tc0 + 512], AF.Square, (R_acc5,), (R_sq5,))
                for j in range(8):
                    P.mm(banks[6], ones_bf, sq5[:, j, :], j == 0, j == 7, (R_sq5, R_const), (RB[6],))
                rstd_from_ss(rstd5, banks[6], 1.0 / D, (RB[6], R_const), (R_rstd5,), lnt5)
                for j in range(8):
                    yb_ = c5["y"] % 2
                    c5["y"] += 1
                    P.stt("dve", yout[yb_], acc5[:, j, tc0:tc0 + 512], g_fin[:, j:j + 1], rstd5, ALU.mult, ALU.mult,
                          (R_acc5, R_rstd5, R_vec), (R_yout[yb_],))
                    P.dma("pool", yT[s][j * 128:(j + 1) * 128, q0:q0 + 512], yout[yb_], (R_yout[yb_],), ())
        A.release(m5)

        with nc.Block() as block:
            P.emit(block, csem, dsem)
    return nc


def _rope_tables(S):
    inv = (np.float32(10000.0) ** (-np.arange(0, 32, 2, dtype=np.float32) / np.float32(32))).astype(np.float32)
    ang = (np.arange(S, dtype=np.float32)[:, None] * inv[None, :]).astype(np.float32)
    cos = np.cos(ang).astype(np.float32).T
    sin = np.sin(ang).astype(np.float32).T
    cs = np.concatenate([cos, cos], axis=0)
    sn = np.concatenate([-sin, sin], axis=0)
    return cs, sn


def _dmask():
    slopes = (2.0 ** (-8.0 * np.arange(1, 9) / 8)).astype(np.float32)
    p = np.arange(128)[:, None]
    f = np.arange(128)[None, :]
    out = np.zeros((128, 48, 128), np.float32)
    for h in range(8):
        for di, d in enumerate(DPAT):
            dA = p - 64 - f
            mA = np.where(p >= f, np.exp(-slopes[h] * np.abs(dA).astype(np.float32) * d), 0.0)
            dB = p + 64 - f
            mB = np.where(p <= f, np.exp(-slopes[h] * np.abs(dB).astype(np.float32) * d), 0.0)
            out[:, (h * 3 + di) * 2 + 0, :] = mA
            out[:, (h * 3 + di) * 2 + 1, :] = mB
    return out.reshape(128, 48 * 128)


_NC_CACHE = {}


def kernel(x_prompt, x_sample, c_prompt, c_sample, ada_w, ada_b, norm_mix_g, w_in, q_norm_g, kv_norm_g,
           w_uq, w_ukv, w_out, norm_moe_g, w_router_group, w_router_expert, w_gate, w_up, w_down,
           final_norm_g, _debug_out=None):
    f = np.float32
    x_prompt = np.asarray(x_prompt, f)
    x_sample = np.asarray(x_sample, f)
    w_in0 = np.ascontiguousarray(np.asarray(w_in, f)[0])
    w_uq0 = np.asarray(w_uq, f)[0]
    pe = w_in0[:, 384:416]
    w_pe_sw = np.ascontiguousarray(np.concatenate([pe[:, 16:32], pe[:, 0:16]], axis=1))
    wq = w_uq0.reshape(256, 8, 96)
    wq_sw = wq.copy()
    wq_sw[:, :, 64:80] = wq[:, :, 80:96]
    wq_sw[:, :, 80:96] = wq[:, :, 64:80]
    w_uq_sw = np.ascontiguousarray(wq_sw.reshape(256, 768))
    w_r = np.ascontiguousarray(np.concatenate([np.asarray(w_router_group, f)[0],
                                               np.asarray(w_router_expert, f)[0].reshape(D, 16)], axis=1))
    cs_full = {"p": _rope_tables(SP), "s": _rope_tables(SS)}
    dmask = _dmask()
    ident = np.eye(128, dtype=f)
    sel = np.zeros((16, NEXP * 128), f)
    for e in range(NEXP):
        sel[e, e * 128:(e + 1) * 128] = 1.0
    shared = {
        "ada_w": np.ascontiguousarray(np.asarray(ada_w, f)[0]),
        "w_in": w_in0, "w_pe_sw": w_pe_sw,
        "w_uq": np.ascontiguousarray(w_uq0), "w_uq_sw": w_uq_sw,
        "w_ukv": np.ascontiguousarray(np.asarray(w_ukv, f)[0]),
        "w_out": np.ascontiguousarray(np.asarray(w_out, f)[0]),
        "w_r": w_r,
        "w_gate": np.ascontiguousarray(np.asarray(w_gate, f)[0]),
        "w_up": np.ascontiguousarray(np.asarray(w_up, f)[0]),
        "w_down": np.ascontiguousarray(np.asarray(w_down, f)[0]),
        "dmask": dmask, "ident": ident, "sel": sel,
    }

    def chunked(v, nch):
        return np.asarray(v, f).reshape(nch, 128).T

    xT_full = {}
    for g in range(2):
        xT_full[("p", g)] = np.ascontiguousarray(x_prompt[g].T)
        xT_full[("s", g)] = np.ascontiguousarray(x_sample[g].T)
    in_maps = []
    for core in range(8):
        g, j = core // 4, core % 4
        m = dict(shared)
        c2 = np.stack([np.asarray(c_prompt, f)[g], np.asarray(c_sample, f)[g]], axis=1)
        vecs = np.zeros((128, 96), f)
        vecs[:, 0:16] = c2.reshape(8, 128, 2).transpose(1, 0, 2).reshape(128, 16)
        vecs[:, 16:64] = chunked(np.asarray(ada_b, f)[0], 48)
        vecs[:, 64:72] = chunked(np.asarray(norm_mix_g, f)[0], 8)
        vecs[:, 72:80] = chunked(np.asarray(norm_moe_g, f)[0], 8)
        vecs[:, 80:88] = chunked(np.asarray(final_norm_g, f), 8)
        vecs[:, 88:90] = chunked(np.asarray(q_norm_g, f)[0], 2)
        vecs[:, 90:91] = chunked(np.asarray(kv_norm_g, f)[0], 1)
        m["vecs"] = vecs
        for s, S_, n in (("p", SP, NP_), ("s", SS, NS_)):
            o = j * n
            idx = (o - HALO + np.arange(S_)) % S_
            m["xT" + s] = np.ascontiguousarray(xT_full[(s, g)][:, idx])
            cs, sn = cs_full[s]
            m["cs_" + s] = np.ascontiguousarray(cs[:, idx])
            m["sn_" + s] = np.ascontiguousarray(sn[:, idx])
            u = o - HALO + np.arange(n + 2 * HALO)
            val = ((u >= 0) & (u < S_)).astype(f)
            m["valid_" + s] = np.ascontiguousarray(val.reshape(-1, 128).T)
        in_maps.append(m)

    if "nc" not in _NC_CACHE:
        _NC_CACHE["nc"] = build_program()
    nc = _NC_CACHE["nc"]
    res = run_bass_kernel_spmd(nc, in_maps, core_ids=list(range(8)))
    y_prompt = np.empty((2, SP, D), f)
    y_sample = np.empty((2, SS, D), f)
    for core in range(8):
        g, j = core // 4, core % 4
        r = res.results[core]
        y_prompt[g, j * NP_:(j + 1) * NP_, :] = np.asarray(r["yTp"]).T
        y_sample[g, j * NS_:(j + 1) * NS_, :] = np.asarray(r["yTs"]).T
    if _debug_out is not None:
        _debug_out.append(res)
    return (y_prompt, y_sample)


if __name__ == "__main__":
    import time
    t = time.time()
    nc = build_program()
    print("build ok", time.time() - t)
```

```python
import numpy as np
import concourse.bass as bass
import concourse.mybir as mybir
from concourse.bass_utils import run_bass_kernel_spmd

F32 = mybir.dt.float32
BF16 = mybir.dt.bfloat16
AF = mybir.ActivationFunctionType
ALU = mybir.AluOpType
AX = mybir.AxisListType

DEBUG = False
STRICT_SAME_ENGINE = True

D = 1024
SP, SS = 16384, 8192
NP_, NS_ = 4096, 2048
HALO = 1024
EPS = 1e-6
NEXP = 16
DPAT = (1, 4, 16)


class Res:
    __slots__ = ("name", "w", "r")

    def __init__(self, name=""):
        self.name = name
        self.w = {}
        self.r = {}


class Prog:
    CENG = ("pe", "act", "dve", "pool")

    def __init__(self, nc):
        self.nc = nc
        self.engs = ["pe", "act", "dve", "pool", "sp"]
        self.ops = {e: [] for e in self.engs}
        self.pending = {e: [] for e in self.engs}
        self.nslots = {"sp": 12, "pool": 8, "act": 4}
        self.dma_count = {q: 0 for q in self.nslots}
        self.dma_tok = {q: [None] * n for q, n in self.nslots.items()}

    def _collect(self, eng, reads, writes, is_dma=False):
        waits = list(self.pending[eng])
        self.pending[eng] = []
        for r in reads:
            for t in r.w.values():
                waits.append(t)
        for w in writes:
            for t in w.w.values():
                if t[0] == "c" and t[1] == eng and not STRICT_SAME_ENGINE:
                    continue
                if is_dma and t[0] == "d":
                    continue
                waits.append(t)
            for t in w.r.values():
                if t[0] == "c" and t[1] == eng and not STRICT_SAME_ENGINE:
                    continue
                waits.append(t)
        if eng == "pe":
            waits = [t for t in waits if not (t[0] == "c" and t[1] == "pe")]
        return waits

    def op(self, eng, fn, reads=(), writes=()):
        waits = self._collect(eng, reads, writes)
        idx = len(self.ops[eng])
        tok = ("c", eng, idx)
        self.ops[eng].append(dict(fn=fn, waits=waits, kind="c"))
        for r in reads:
            r.r[eng] = tok
        for w in writes:
            w.w[eng] = tok
        return tok

    def dma(self, q, out, in_, reads=(), writes=()):
        waits = self._collect(q, reads, writes, is_dma=True)
        k = self.dma_count[q]
        ns = self.nslots[q]
        slot = k % ns
        value = 16 * (k // ns + 1)
        self.dma_count[q] = k + 1
        if self.dma_tok[q][slot] is not None:
            waits.append(self.dma_tok[q][slot])
        tok = ("d", q, slot, value)
        self.dma_tok[q][slot] = tok
        self.ops[q].append(dict(fn=lambda e: e.dma_start(out=out, in_=in_), waits=waits, kind="d", q=q, slot=slot))
        key = (q, slot)
        for r in reads:
            r.r[key] = tok
        for w in writes:
            w.w[key] = tok
        return tok

    def barrier(self):
        toks = []
        for e in self.CENG:
            n = len(self.ops[e])
            for i in range(n - 1, -1, -1):
                if self.ops[e][i]["kind"] == "c":
                    toks.append(("c", e, i))
                    break
        for q in self.nslots:
            for t in self.dma_tok[q]:
                if t is not None:
                    toks.append(t)
        for e in self.engs:
            self.pending[e].extend(toks)

    def emit(self, block, csem, dsem):
        needed = {e: set() for e in self.CENG}
        for e in self.engs:
            for o in self.ops[e]:
                for t in o["waits"]:
                    if t[0] == "c":
                        needed[t[1]].add(t[2])
        final = []
        for q in self.nslots:
            for t in self.dma_tok[q]:
                if t is not None:
                    final.append(t)
        rank = {e: {idx: i + 1 for i, idx in enumerate(sorted(needed[e]))} for e in self.CENG}

        def body(ename):
            def f(eh):
                waited = {}

                def do_wait(t):
                    if t[0] == "c":
                        sem = csem[t[1]]
                        val = rank[t[1]][t[2]]
                        key = t[1]
                    else:
                        sem = dsem[t[1]][t[2]]
                        val = t[3]
                        key = (t[1], t[2])
                    if waited.get(key, 0) >= val:
                        return
                    eh.wait_ge(sem, val)
                    waited[key] = val

                for idx, o in enumerate(self.ops[ename]):
                    for t in o["waits"]:
                        do_wait(t)
                    ins = o["fn"](eh)
                    if o["kind"] == "c":
                        if ename in rank and idx in rank[ename]:
                            ins.then_inc(csem[ename], 1)
                    else:
                        ins.then_inc(dsem[o["q"]][o["slot"]], 16)
                if ename == "sp":
                    for t in final:
                        do_wait(t)
            return f

        block.tensor(body("pe"))
        block.scalar(body("act"))
        block.vector(body("dve"))
        block.gpsimd(body("pool"))
        block.sync(body("sp"))

    def mm(self, out, lhsT, rhs, start, stop, R, W):
        self.op("pe", lambda e: e.matmul(out, lhsT=lhsT, rhs=rhs, start=start, stop=stop), R, W)

    def tr(self, out, in_, ident, R, W):
        self.op("pe", lambda e: e.transpose(out, in_, ident), R, W)

    def act(self, out, in_, func, R, W, bias=None, scale=None):
        kw = {}
        if bias is not None:
            kw["bias"] = bias
        if scale is not None:
            kw["scale"] = scale
        self.op("act", lambda e: e.activation(out=out, in_=in_, func=func, **kw), R, W)

    def tt(self, eng, out, in0, in1, op, R, W):
        self.op(eng, lambda e: e.tensor_tensor(out=out, in0=in0, in1=in1, op=op), R, W)

    def ts(self, eng, out, in0, s1, op0, R, W, s2=None, op1=None):
        if op1 is None:
            self.op(eng, lambda e: e.tensor_scalar(out=out, in0=in0, scalar1=s1, scalar2=None, op0=op0), R, W)
        else:
            self.op(eng, lambda e: e.tensor_scalar(out=out, in0=in0, scalar1=s1, scalar2=s2, op0=op0, op1=op1), R, W)

    def stt(self, eng, out, in0, scalar, in1, op0, op1, R, W):
        self.op(eng, lambda e: e.scalar_tensor_tensor(out=out, in0=in0, scalar=scalar, in1=in1, op0=op0, op1=op1), R, W)

    def copy(self, eng, out, in_, R, W):
        if eng == "act":
            self.op("act", lambda e: e.copy(out=out, in_=in_), R, W)
        else:
            self.op(eng, lambda e: e.tensor_copy(out=out, in_=in_), R, W)

    def memset(self, eng, ap, val, W):
        self.op(eng, lambda e: e.memset(ap, val), (), W)

    def recip(self, out, in_, R, W):
        self.op("dve", lambda e: e.reciprocal(out=out, in_=in_), R, W)

    def reduce(self, out, in_, op, R, W):
        self.op("dve", lambda e: e.tensor_reduce(out=out, in_=in_, axis=AX.X, op=op), R, W)


class Arena:
    def __init__(self, t, nfloats):
        self.t = t
        self.n = nfloats * 4
        self.off = 0

    def mark(self):
        return self.off

    def release(self, m):
        self.off = m

    def f32(self, n, parts=128):
        o = (self.off + 31) // 32 * 32
        assert o + 4 * n <= self.n, f"arena overflow {o + 4 * n} > {self.n}"
        self.off = o + 4 * n
        return self.t[0:parts, o // 4:o // 4 + n]

    def bf(self, n, parts=128):
        nn = (n + 1) // 2
        return self.f32(nn, parts).bitcast(BF16)[:, 0:n]


def build_program():
    nc = bass.Bass("TRN2", target_bir_lowering=False)
    skind = "ExternalOutput" if DEBUG else "Internal"

    def din(name, shape, dt=F32):
        return nc.dram_tensor(name, list(shape), dt, kind="ExternalInput").ap()

    def dscr(name, shape, dt=BF16):
        return nc.dram_tensor(name, list(shape), dt, kind=skind).ap()

    xT = {"p": din("xTp", [D, SP]), "s": din("xTs", [D, SS])}
    vecs = din("vecs", [128, 96])
    ada_w = din("ada_w", [D, 6 * D])
    w_in = din("w_in", [D, 1952])
    w_pe_sw = din("w_pe_sw", [D, 32])
    w_uq = din("w_uq", [256, 768])
    w_uq_sw = din("w_uq_sw", [256, 768])
    w_ukv = din("w_ukv", [128, 1024])
    w_out = din("w_out", [D, D])
    w_r = din("w_r", [D, 20])
    w_gate = din("w_gate", [NEXP, D, 512])
    w_up = din("w_up", [NEXP, D, 512])
    w_down = din("w_down", [NEXP, 512, D])
    cs_in = {"p": din("cs_p", [32, SP]), "s": din("cs_s", [32, SS])}
    sn_in = {"p": din("sn_p", [32, SP]), "s": din("sn_s", [32, SS])}
    valid_in = {"p": din("valid_p", [128, 48]), "s": din("valid_s", [128, 32])}
    dmask_in = din("dmask", [128, 48 * 128])
    ident_in = din("ident", [128, 128])
    sel_in = din("sel", [16, NEXP * 128])

    yT = {"p": nc.dram_tensor("yTp", [D, NP_], F32, kind="ExternalOutput").ap(),
          "s": nc.dram_tensor("yTs", [D, NS_], F32, kind="ExternalOutput").ap()}

    SEQ = {"p": SP, "s": SS}
    NOWN = {"p": NP_, "s": NS_}
    SIDX = {"p": 0, "s": 1}
    ckvn_d = {s: dscr("ckvn_" + s, [128, SEQ[s]]) for s in "ps"}
    kpe_d = {s: dscr("kpe_" + s, [32, SEQ[s]]) for s in "ps"}
    qm_d = {s: dscr("qm_" + s, [8, 96, NOWN[s]]) for s in "ps"}
    qd_d = {s: dscr("qd_" + s, [512, NOWN[s]]) for s in "ps"}
    kd_d = {s: dscr("kd_" + s, [512, NOWN[s] + 2 * HALO]) for s in "ps"}
    vd_d = {s: dscr("vd_" + s, [NOWN[s] + 2 * HALO, 520]) for s in "ps"}
    ya_d = {s: dscr("ya_" + s, [512, NOWN[s]]) for s in "ps"}
    yb_d = {s: dscr("yb_" + s, [512, NOWN[s]]) for s in "ps"}
    R_ckvn = {s: Res() for s in "ps"}
    R_kpe = {s: Res() for s in "ps"}
    R_qm = {s: Res() for s in "ps"}
    R_qd = {s: Res() for s in "ps"}
    R_kd = {s: Res() for s in "ps"}
    R_vd = {s: Res() for s in "ps"}
    R_ya = {s: Res() for s in "ps"}
    R_yb = {s: Res() for s in "ps"}

    NARENA = 52000
    from contextlib import ExitStack
    with ExitStack() as es:
        arena_t = es.enter_context(nc.sbuf_tensor("arena", [128, NARENA], F32))
        tri = [es.enter_context(nc.psum_tensor(f"tri{i}", [128, 1536], F32)) for i in range(2)]
        tri = [p_[:, :] for p_ in tri]
        pair_ = es.enter_context(nc.psum_tensor("pair", [128, 1024], F32))[:, :]
        banks = [tri[i // 3][:, (i % 3) * 512:(i % 3 + 1) * 512] for i in range(6)]
        banks += [pair_[:, 0:512], pair_[:, 512:1024]]
        RB = [Res(f"bank{i}") for i in range(8)]
        P = Prog(nc)
        csem = {e: es.enter_context(nc.semaphore("c_" + e)) for e in Prog.CENG}
        dsem = {q: [es.enter_context(nc.semaphore(f"d_{q}{i}")) for i in range(n)] for q, n in P.nslots.items()}
        A = Arena(arena_t, NARENA)

        vec = A.f32(96)
        R_vec = Res()
        P.dma("sp", vec, vecs, (), (R_vec,))
        cT = vec[:, 0:16].rearrange("p (j s) -> p j s", s=2)
        ada_b = vec[:, 16:64]
        g_mix = vec[:, 64:72]
        g_moe = vec[:, 72:80]
        g_fin = vec[:, 80:88]
        qg = vec[:, 88:90]
        kvg = vec[:, 90:91]
        ones_bf = A.bf(128)
        ones32 = A.f32(64)
        R_const = Res()
        P.memset("dve", ones_bf, 1.0, (R_const,))
        P.memset("dve", ones32, 1.0, (R_const,))
        ident = A.f32(128)
        P.dma("sp", ident, ident_in, (), (R_const,))
        mod = A.f32(96).rearrange("p (m s) -> p m s", s=2)
        g1s = A.f32(16).rearrange("p (j s) -> p j s", s=2)
        g2s = A.f32(16).rearrange("p (j s) -> p j s", s=2)
        R_mod = Res()

        m0 = A.mark()
        sc = A.f32(16).rearrange("p (j s) -> p j s", s=2)
        R_sc = Res()
        P.act(sc, cT, AF.Silu, (R_vec,), (R_sc,))
        adaw_v = ada_w.rearrange("(j p) c -> p j c", p=128)
        pieces = [A.f32(8 * 512).rearrange("p (j c) -> p j c", c=512) for _ in range(3)]
        R_piece = [Res() for _ in range(3)]
        modT = A.f32(6144)
        R_modT = Res()
        for q in range(12):
            pc = pieces[q % 3]
            P.dma("sp", pc, adaw_v[:, :, q * 512:(q + 1) * 512], (), (R_piece[q % 3],))
            bk = 1 + q % 3
            for j in range(8):
                P.mm(banks[bk][0:2, :], sc[:, j, :], pc[:, j, :], j == 0, j == 7, (R_piece[q % 3], R_sc), (RB[bk],))
            P.copy("act" if q % 2 == 0 else "dve", modT[0:2, q * 512:(q + 1) * 512], banks[bk][0:2, :], (RB[bk],), (R_modT,))
        for m in range(48):
            P.mm(banks[0][:, 2 * m:2 * m + 2], modT[0:2, m * 128:(m + 1) * 128], ident[0:2, 0:2], True, True,
                 (R_modT, R_const), (RB[0],))
        P.tt("dve", mod, banks[0][:, 0:96].rearrange("p (m s) -> p m s", s=2),
             ada_b.unsqueeze(2).broadcast_to([128, 48, 2]), ALU.add, (RB[0], R_vec), (R_mod,))
        P.ts("dve", g1s, mod[:, 8:16, :], 1.0, ALU.add, (R_mod,), (R_mod,))
        P.tt("dve", g1s, g1s, g_mix.unsqueeze(2).broadcast_to([128, 8, 2]), ALU.mult, (R_mod, R_vec), (R_mod,))
        P.ts("dve", g2s, mod[:, 32:40, :], 1.0, ALU.add, (R_mod,), (R_mod,))
        P.tt("dve", g2s, g2s, g_moe.unsqueeze(2).broadcast_to([128, 8, 2]), ALU.mult, (R_mod, R_vec), (R_mod,))
        P.barrier()
        A.release(m0)

        def rstd_from_ss(out_sb, ss_ps, inv_n, R, W, tmp):
            P.act(tmp, ss_ps, AF.Ln, R, W, bias=eps_ap, scale=inv_n)
            P.act(out_sb, tmp, AF.Exp, W, W, scale=-0.5)

        eps_t = A.f32(1)
        eps_ap = eps_t[:, 0:1]
        P.memset("dve", eps_ap, EPS, (R_const,))

        m1 = A.mark()
        R_w1 = Res()
        wib = A.bf(8 * 1952).rearrange("p (j c) -> p j c", c=1952)
        wpsw = A.bf(8 * 32).rearrange("p (j c) -> p j c", c=32)
        wuq = A.bf(2 * 768).rearrange("p (j c) -> p j c", c=768)
        wuqs = A.bf(2 * 768).rearrange("p (j c) -> p j c", c=768)
        mst = A.mark()
        stg = [A.f32(1952) for _ in range(2)]
        R_stg = [Res(), Res()]
        k = 0
        for j in range(8):
            P.dma("sp", stg[k % 2], w_in[j * 128:(j + 1) * 128, :], (), (R_stg[k % 2],))
            P.copy("dve" if k % 2 == 0 else "pool", wib[:, j, :], stg[k % 2], (R_stg[k % 2],), (R_w1,))
            k += 1
        P.dma("sp", stg[k % 2][:, 0:256].rearrange("p (j c) -> p j c", c=32),
              w_pe_sw.rearrange("(j p) c -> p j c", p=128), (), (R_stg[k % 2],))
        P.copy("dve", wpsw, stg[k % 2][:, 0:256].rearrange("p (j c) -> p j c", c=32), (R_stg[k % 2],), (R_w1,))
        k += 1
        for (src, dst) in ((w_uq, wuq), (w_uq_sw, wuqs)):
            P.dma("sp", stg[k % 2][:, 0:1536].rearrange("p (j c) -> p j c", c=768),
                  src.rearrange("(j p) c -> p j c", p=128), (), (R_stg[k % 2],))
            P.copy("dve", dst, stg[k % 2][:, 0:1536].rearrange("p (j c) -> p j c", c=768), (R_stg[k % 2],), (R_w1,))
            k += 1
        C_CQ, C_KV, C_PE, C_QD, C_KD, C_VD = 0, 256, 384, 416, 928, 1440

        xt = [A.f32(8 * 512).rearrange("p (j c) -> p j c", c=512) for _ in range(3)]
        R_x = [Res(), Res(), Res()]
        sq = A.bf(8 * 512).rearrange("p (j c) -> p j c", c=512)
        R_sq = Res()
        h32 = A.f32(4 * 512).rearrange("p (j c) -> p j c", c=512)
        R_h32 = Res()
        hb = [A.bf(8 * 512).rearrange("p (j c) -> p j c", c=512) for _ in range(2)]
        R_hb = [Res(), Res()]
        rstd = A.f32(512)
        lnt = A.f32(512)
        R_rstd = Res()
        rstd2 = A.f32(512)
        lnt2 = A.f32(512)
        R_rstd2 = Res()
        sq2 = A.bf(2 * 512).rearrange("p (j c) -> p j c", c=512)
        R_sq2 = Res()
        ckvn_sb = [A.bf(512) for _ in range(2)]
        R_ckvn_sb = [Res(), Res()]
        cst = [A.f32(512, 32) for _ in range(2)]
        snt = [A.f32(512, 32) for _ in range(2)]
        R_cs = [Res(), Res()]
        t1 = A.f32(512)
        t2 = A.f32(512)
        R_t12 = Res()
        kper = [A.bf(512, 32) for _ in range(2)]
        R_kper = [Res(), Res()]
        ev = [A.bf(4 * 512).rearrange("p (j c) -> p j c", c=512) for _ in range(2)]
        R_ev = [Res() for _ in range(2)]
        vev = [A.bf(4 * 520).rearrange("p (j c) -> p j c", c=520) for _ in range(2)]
        R_vev = [Res(), Res()]
        cqn = A.bf(2 * 512).rearrange("p (j c) -> p j c", c=512)
        R_cqn = Res()
        qev = [A.bf(512) for _ in range(2)]
        R_qev = [Res(), Res()]
        csq = [A.f32(512) for _ in range(2)]
        snq = [A.f32(512) for _ in range(2)]
        R_csq = [Res(), Res()]
        valid = {s: A.f32(48) for s in "ps"}
        R_valid = Res()
        P.dma("sp", valid["p"], valid_in["p"], (), (R_valid,))
        P.dma("sp", valid["s"][:, 0:32], valid_in["s"], (), (R_valid,))
        for b in range(2):
            P.memset("pool", vev[b], 0.0, (R_vev[b],))

        cnt = {"ev": 0, "vev": 0, "qev": 0, "rot": 0}
        ROT = [5, 6, 7]

        def nextbank():
            b = ROT[cnt["rot"] % 3]
            cnt["rot"] += 1
            return b

        tiles = []
        for s in "ps":
            nt_ = SEQ[s] // 512
            nh_ = (NOWN[s] + 2 * HALO) // 512
            A_ = list(range(nh_))
            B_ = list(range(nh_, nt_))
            ia = ib = 0
            while ia < len(A_) or ib < len(B_):
                if ib >= len(B_) or (ia < len(A_) and ia * len(B_) <= ib * len(A_)):
                    tiles.append((s, A_[ia]))
                    ia += 1
                else:
                    tiles.append((s, B_[ib]))
                    ib += 1

        rstd_b = [rstd, A.f32(512)]
        R_h32j = [Res() for _ in range(8)]
        R_rstd_b = [R_rstd, Res()]

        def stageA1(i):
            s, t = tiles[i]
            b = i % 2
            x3 = i % 3
            c0 = t * 512
            P.dma("sp", xt[x3], xT[s].rearrange("(j p) c -> p j c", p=128)[:, :, c0:c0 + 512], (), (R_x[x3],))
            P.act(sq, xt[x3], AF.Square, (R_x[x3],), (R_sq,))
            yield
            for j in range(8):
                P.mm(banks[0], ones_bf, sq[:, j, :], j == 0, j == 7, (R_sq, R_const), (RB[0],))
            yield
            P.act(lnt, banks[0], AF.Ln, (RB[0], R_const), (R_rstd_b[b],), bias=eps_ap, scale=1.0 / D)
            yield
            P.act(rstd_b[b], lnt, AF.Exp, (R_rstd_b[b],), (R_rstd_b[b],), scale=-0.5)
            yield

        def stageB1(i):
            s, t = tiles[i]
            si = SIDX[s]
            b = i % 2
            x3 = i % 3
            for j in range(8):
                j4 = j % 4
                P.tt("dve", h32[:, j4, :], xt[x3][:, j, :], rstd_b[b], ALU.mult, (R_x[x3], R_rstd_b[b]), (R_h32j[j4],))
                if j % 2 == 0:
                    P.act(hb[b][:, j, :], h32[:, j4, :], AF.Identity, (R_h32j[j4], R_mod), (R_hb[b],),
                          bias=mod[:, j, si:si + 1], scale=g1s[:, j, si:si + 1])
                else:
                    P.ts("dve", hb[b][:, j, :], h32[:, j4, :], g1s[:, j, si:si + 1], ALU.mult, (R_h32j[j4], R_mod), (R_hb[b],),
                         s2=mod[:, j, si:si + 1], op1=ALU.add)
                yield

        def proj(bank, wcols, b, M=128, w=None):
            w = wib if w is None else w
            for j in range(8):
                P.mm(banks[bank][0:M, :], w[:, j, wcols:wcols + M], hb[b][:, j, :], j == 0, j == 7,
                     (R_hb[b], R_w1), (RB[bank],))

        def main(i):
            s, t = tiles[i]
            si = SIDX[s]
            b = i % 2
            c0 = t * 512
            n = NOWN[s]
            proj(1, C_KV, b)
            yield
            proj(2, C_PE, b, M=32)
            proj(3, 0, b, M=32, w=wpsw)
            yield
            P.dma("sp", cst[b], cs_in[s][:, c0:c0 + 512], (), (R_cs[b],))
            P.dma("sp", snt[b], sn_in[s][:, c0:c0 + 512], (), (R_cs[b],))
            P.act(sq2[:, 0, :], banks[1], AF.Square, (RB[1],), (R_sq2,))
            P.mm(banks[4], ones_bf, sq2[:, 0, :], True, True, (R_sq2, R_const), (RB[4],))
            rstd_from_ss(rstd2, banks[4], 1.0 / 128, (RB[4], R_const), (R_rstd2,), lnt2)
            yield
            P.stt("dve", ckvn_sb[b], banks[1], kvg, rstd2, ALU.mult, ALU.mult, (RB[1], R_rstd2, R_vec), (R_ckvn_sb[b],))
            P.dma("pool", ckvn_d[s][:, c0:c0 + 512], ckvn_sb[b], (R_ckvn_sb[b],), (R_ckvn[s],))
            yield
            P.tt("dve", t1[0:32, :], banks[2][0:32, :], cst[b], ALU.mult, (RB[2], R_cs[b]), (R_t12,))
            P.tt("dve", t2[0:32, :], banks[3][0:32, :], snt[b], ALU.mult, (RB[3], R_cs[b]), (R_t12,))
            P.tt("dve", kper[b], t1[0:32, :], t2[0:32, :], ALU.add, (R_t12,), (R_kper[b],))
            P.dma("pool", kpe_d[s][:, c0:c0 + 512], kper[b], (R_kper[b],), (R_kpe[s],))
            yield
            if c0 < n + 2 * HALO:
                e = cnt["ev"] % 2
                cnt["ev"] += 1
                for c in range(4):
                    bk = nextbank()
                    proj(bk, C_KD + c * 128, b)
                    P.copy("act" if c % 2 == 0 else "dve", ev[e][:, c, :], banks[bk], (RB[bk],), (R_ev[e],))
                    yield
                P.dma("pool", kd_d[s].rearrange("(c p) n -> p c n", p=128)[:, :, c0:c0 + 512], ev[e], (R_ev[e],), (R_kd[s],))
                v = cnt["vev"] % 2
                cnt["vev"] += 1
                for tc in range(4):
                    bk = nextbank()
                    for j in range(8):
                        P.mm(banks[bk], hb[b][:, j, tc * 128:(tc + 1) * 128], wib[:, j, C_VD:C_VD + 512], j == 0, j == 7,
                             (R_hb[b], R_w1), (RB[bk],))
                    vcol = valid[s][:, t * 4 + tc:t * 4 + tc + 1]
                    P.ts("dve", vev[v][:, tc, :].rearrange("p (h c) -> p h c", c=65)[:, :, 0:64],
                         banks[bk].rearrange("p (h c) -> p h c", c=64), vcol, ALU.mult, (RB[bk], R_valid), (R_vev[v],))
                    P.copy("pool", vev[v][:, tc, :].rearrange("p (h c) -> p h c", c=65)[:, :, 64:65],
                           vcol.unsqueeze(1).broadcast_to([128, 8, 1]), (R_valid,), (R_vev[v],))
                    yield
                P.dma("pool", vd_d[s][c0:c0 + 512, :].rearrange("(c p) f -> p c f", p=128), vev[v], (R_vev[v],), (R_vd[s],))
            if HALO <= c0 < HALO + n:
                q0 = c0 - HALO
                e = cnt["ev"] % 2
                cnt["ev"] += 1
                for c in range(4):
                    bk = nextbank()
                    proj(bk, C_QD + c * 128, b)
                    P.copy("act" if c % 2 == 0 else "dve", ev[e][:, c, :], banks[bk], (RB[bk],), (R_ev[e],))
                    yield
                P.dma("pool", qd_d[s].rearrange("(c p) n -> p c n", p=128)[:, :, q0:q0 + 512], ev[e], (R_ev[e],), (R_qd[s],))
                bks = [nextbank(), nextbank()]
                for c in range(2):
                    proj(bks[c], C_CQ + c * 128, b)
                    P.act(sq2[:, c, :], banks[bks[c]], AF.Square, (RB[bks[c]],), (R_sq2,))
                for c in range(2):
                    P.mm(banks[4], ones_bf, sq2[:, c, :], c == 0, c == 1, (R_sq2, R_const), (RB[4],))
                rstd_from_ss(rstd2, banks[4], 1.0 / 256, (RB[4], R_const), (R_rstd2,), lnt2)
                yield
                for c in range(2):
                    P.stt("dve", cqn[:, c, :], banks[bks[c]], qg[:, c:c + 1], rstd2, ALU.mult, ALU.mult,
                          (RB[bks[c]], R_rstd2, R_vec), (R_cqn,))
                P.dma("sp", csq[b][64:96, :], cs_in[s][:, c0:c0 + 512], (), (R_csq[b],))
                P.dma("sp", snq[b][64:96, :], sn_in[s][:, c0:c0 + 512], (), (R_csq[b],))
                for h in range(8):
                    ba = nextbank()
                    bb = nextbank()
                    for c in range(2):
                        P.mm(banks[ba][0:96, :], wuq[:, c, h * 96:(h + 1) * 96], cqn[:, c, :], c == 0, c == 1,
                             (R_cqn, R_w1), (RB[ba],))
                    for c in range(2):
                        P.mm(banks[bb][0:96, :], wuqs[:, c, h * 96:(h + 1) * 96], cqn[:, c, :], c == 0, c == 1,
                             (R_cqn, R_w1), (RB[bb],))
                    qe = cnt["qev"] % 2
                    cnt["qev"] += 1
                    P.copy("act", qev[qe][0:64, :], banks[ba][0:64, :], (RB[ba],), (R_qev[qe],))
                    P.tt("dve", t1[64:96, :], banks[ba][64:96, :], csq[b][64:96, :], ALU.mult, (RB[ba], R_csq[b]), (R_t12,))
                    P.tt("dve", t2[64:96, :], banks[bb][64:96, :], snq[b][64:96, :], ALU.mult, (RB[bb], R_csq[b]), (R_t12,))
                    P.tt("dve", qev[qe][64:96, :], t1[64:96, :], t2[64:96, :], ALU.add, (R_t12,), (R_qev[qe],))
                    P.dma("pool", qm_d[s][h, :, q0:q0 + 512], qev[qe][0:96, :], (R_qev[qe],), (R_qm[s],))
                    yield

        NT = len(tiles)

        def _empty():
            return
            yield

        for i in range(NT + 2):
            gens = [main(i - 2) if 2 <= i else _empty(),
                    stageB1(i - 1) if 1 <= i <= NT else _empty(),
                    stageA1(i) if i < NT else _empty()]
            done = [False, False, False]
            while not all(done):
                for gi in range(3):
                    if not done[gi]:
                        try:
                            next(gens[gi])
                        except StopIteration:
                            done[gi] = True
        P.barrier()
        A.release(m1)

        def finalize(o_sb, ncols, out_bf, bank, R_o, R_out, rden, R_rden):
            P.recip(rden[64:65, 0:ncols], o_sb[64:65, 0:ncols], (R_o,), (R_rden,))
            P.mm(banks[bank][0:64, 0:ncols], ones32[64:65, 0:64], rden[64:65, 0:ncols], True, True,
                 (R_rden, R_const), (RB[bank],))
            P.tt("dve", out_bf, o_sb[0:64, 0:ncols], banks[bank][0:64, 0:ncols], ALU.mult, (R_o, RB[bank]), (R_out,))

        m3 = A.mark()
        wkv_st = A.f32(1024)
        wkv = A.bf(1024)
        R_wkv = Res()
        P.dma("sp", wkv_st, w_ukv, (), (R_wkv,))
        P.copy("dve", wkv, wkv_st, (R_wkv,), (R_wkv,))
        ckvn = A.bf(SP)
        R_ck = Res()
        Kt = [A.bf(SP) for _ in range(2)]
        R_K = [Res(), Res()]
        Vt = [A.bf(128 * 65).rearrange("p (c f) -> p c f", f=65) for _ in range(2)]
        R_V = [Res(), Res()]
        Qh = [A.bf(NP_) for _ in range(2)]
        R_Q = [Res(), Res()]
        pT = [A.bf(1536) for _ in range(3)]
        R_pT = [Res() for _ in range(3)]
        o_sb = [A.f32(512) for _ in range(2)]
        R_osb = [Res(), Res()]
        rden = A.f32(512)
        R_rden = Res()
        ybf = [A.bf(512) for _ in range(2)]
        R_ybf = [Res(), Res()]
        for b in range(2):
            P.memset("pool", Vt[b][:, :, 64:65], 1.0, (R_V[b],))
        SCALE_A = 96.0 ** -0.5
        O_B = [6, 6]
        X_B = [7, 7]
        gcnt = {"st": 0, "o": 0, "x": 0, "y": 0, "pt": 0}
        for s in "ps":
            S_ = SEQ[s]
            n = NOWN[s]
            nkc = S_ // 128
            P.dma("sp", ckvn[:, 0:S_], ckvn_d[s], (R_ckvn[s],), (R_ck,))
            for b in range(2):
                P.dma("sp", Kt[b][64:96, 0:S_], kpe_d[s], (R_kpe[s],), (R_K[b],))
            def expand(h):
                kb = h % 2
                for t in range(S_ // 512):
                    xb = X_B[gcnt["x"] % 2]
                    gcnt["x"] += 1
                    P.mm(banks[xb][0:64, :], wkv[:, h * 128:h * 128 + 64], ckvn[:, t * 512:(t + 1) * 512], True, True,
                         (R_wkv, R_ck), (RB[xb],))
                    P.copy("dve", Kt[kb][0:64, t * 512:(t + 1) * 512], banks[xb][0:64, :], (RB[xb],), (R_K[kb],))
                    yield
                for t in range(S_ // 1024):
                    xb = X_B[gcnt["x"] % 2]
                    gcnt["x"] += 1
                    for c in range(8):
                        kc = t * 8 + c
                        P.mm(banks[xb][:, c * 64:(c + 1) * 64], ckvn[:, kc * 128:(kc + 1) * 128],
                             wkv[:, h * 128 + 64:h * 128 + 128], True, True, (R_wkv, R_ck), (RB[xb],))
                    P.copy("dve", Vt[kb][:, t * 8:(t + 1) * 8, 0:64], banks[xb].rearrange("p (c f) -> p c f", f=64),
                           (RB[xb],), (R_V[kb],))
                    yield

            for _ in expand(0):
                pass
            for h in range(8):
                kb = h % 2
                gen_next = expand(h + 1) if h + 1 < 8 else None
                nsteps = (S_ // 512 + S_ // 1024)
                tot_iters = (n // 512) * ((nkc + 2) // 3)
                every = max(1, tot_iters // (nsteps + 1))
                itc = 0
                P.dma("sp", Qh[kb][0:96, 0:n], qm_d[s][h], (R_qm[s],), (R_Q[kb],))
                for qt in range(n // 512):
                    ob = O_B[gcnt["o"] % 2]
                    gcnt["o"] += 1
                    qs = Qh[kb][0:96, qt * 512:(qt + 1) * 512]

                    groups = []
                    kc_ = 0
                    while kc_ < nkc:
                        g_ = min(3, nkc - kc_)
                        groups.append((kc_, g_))
                        kc_ += g_
                    ngr = len(groups)

                    def S_grp(gi):
                        k0, g_ = groups[gi]
                        sp_ = gcnt["st"] % 2
                        gcnt["st"] += 1
                        for u in range(g_):
                            kc = k0 + u
                            P.mm(banks[3 * sp_ + u], Kt[kb][0:96, kc * 128:(kc + 1) * 128], qs, True, True,
                                 (R_K[kb], R_Q[kb]), (RB[3 * sp_ + u],))
                        return sp_
                    sps = {0: S_grp(0), 1: S_grp(1)}
                    for k in range(ngr):
                        itc += 1
                        if gen_next is not None and itc % every == 0:
                            try:
                                next(gen_next)
                            except StopIteration:
                                gen_next = None
                        k0, g_ = groups[k]
                        sp_ = sps.pop(k)
                        pb = gcnt["pt"] % 3
                        gcnt["pt"] += 1
                        P.act(pT[pb][:, 0:g_ * 512], tri[sp_][:, 0:g_ * 512], AF.Exp,
                              tuple(RB[3 * sp_ + u] for u in range(g_)), (R_pT[pb],), scale=SCALE_A)
                        if k + 2 < ngr:
                            sps[k + 2] = S_grp(k + 2)
                        for u in range(g_):
                            kc = k0 + u
                            P.mm(banks[ob][0:65, :], Vt[kb][:, kc, :], pT[pb][:, u * 512:(u + 1) * 512], kc == 0, kc == nkc - 1,
                                 (R_V[kb], R_pT[pb]), (RB[ob],))
                    yb_ = gcnt["y"] % 2
                    gcnt["y"] += 1
                    P.copy("dve", o_sb[yb_][0:65, :], banks[ob][0:65, :], (RB[ob],), (R_osb[yb_],))
                    finalize(o_sb[yb_], 512, ybf[yb_][0:64, :], 7, R_osb[yb_], R_ybf[yb_], rden, R_rden)
                    P.dma("pool", ya_d[s][h * 64:(h + 1) * 64, qt * 512:(qt + 1) * 512], ybf[yb_][0:64, :],
                          (R_ybf[yb_],), (R_ya[s],))
                if gen_next is not None:
                    for _ in gen_next:
                        pass
        P.barrier()
        A.release(m3)

        m4 = A.mark()
        dmask32 = A.f32(48 * 128).rearrange("p (m f) -> p m f", f=128)
        dmask = A.bf(48 * 128).rearrange("p (m f) -> p m f", f=128)
        R_dm = Res()
        P.dma("sp", dmask32, dmask_in.rearrange("p (m f) -> p m f", f=128), (), (R_dm,))
        P.copy("dve", dmask, dmask32, (R_dm,), (R_dm,))
        NBLK = {s: [NOWN[s] // 128 + d for d in DPAT] for s in "ps"}
        NBT = sum(NBLK["p"])
        Vd = [A.bf(NBT * 130).rearrange("p (c f) -> p c f", f=130) for _ in range(2)]
        R_Vd = [Res(), Res()]
        qTd = [A.bf(NP_) for _ in range(2)]
        kTd = [A.bf(NP_ + 2 * HALO) for _ in range(2)]
        R_qk = [Res(), Res()]
        accd = [A.f32(NP_) for _ in range(2)]
        R_acc = [Res(), Res()]
        ebf = [A.bf(512) for _ in range(4)]
        R_ebf = [Res() for _ in range(4)]
        pTd = [A.bf(512) for _ in range(4)]
        R_pTd = [Res() for _ in range(4)]
        lnb = A.f32(512)
        rb4 = A.f32(512)
        R_fin4 = Res()
        ybf4 = [A.bf(512) for _ in range(2)]
        R_ybf4 = [Res(), Res()]
        SA_B = [0, 1, 2, 3]
        OD_B = [4, 5]
        c4 = {"o": 0, "y": 0, "hp": 0, "h": 0, "g": 0}
        items = []
        for s in "ps":
            n = NOWN[s]
            for hp in range(4):
                for hh in range(2):
                    for di, d in enumerate(DPAT):
                        pairs = []
                        if d == 1:
                            for qt in range(n // 128):
                                pairs.append((0, qt))
                        else:
                            for qt in range(n // (128 * d)):
                                for c in range(d):
                                    pairs.append((c, qt))
                        ng = len(pairs) // 4
                        for gi in range(ng):
                            items.append(dict(s=s, hp=hp, hh=hh, di=di, d=d, grp=pairs[gi * 4:gi * 4 + 4],
                                              first_pair=(hh == 0 and di == 0 and gi == 0),
                                              first_head=(di == 0 and gi == 0),
                                              last_head=(di == 2 and gi == ng - 1)))
        state = {}

        def stageA(it):
            s = it["s"]
            n = NOWN[s]
            d = it["d"]
            di = it["di"]
            hp = it["hp"]
            hh = it["hh"]
            h = hp * 2 + hh
            if it["first_pair"]:
                vb = c4["hp"] % 2
                c4["hp"] += 1
                state["vb"] = vb
                boff = []
                o_ = 0
                for d2 in DPAT:
                    boff.append(o_)
                    nb2 = n // (128 * d2) + 1
                    for c in range(d2):
                        r0 = c + HALO - 64 * d2
                        src = vd_d[s][r0:r0 + d2 * (128 * nb2 - 1) + 1:d2, hp * 130:(hp + 1) * 130]
                        src = src.rearrange("(k p) f -> p k f", p=128)
                        P.dma("sp", Vd[vb][:, o_ + c * nb2:o_ + (c + 1) * nb2, :], src, (R_vd[s],), (R_Vd[vb],))
                    o_ += nb2 * d2
                state["boff"] = boff
            if it["first_head"]:
                hb_ = c4["h"] % 2
                c4["h"] += 1
                state["hb"] = hb_
                P.dma("sp", qTd[hb_][0:64, 0:n], qd_d[s][h * 64:(h + 1) * 64, :], (R_qd[s],), (R_qk[hb_],))
                P.dma("sp", kTd[hb_][0:64, 0:n + 2 * HALO], kd_d[s][h * 64:(h + 1) * 64, :], (R_kd[s],), (R_qk[hb_],))
            hb_ = state["hb"]
            vb = state["vb"]
            g = c4["g"] % 2
            c4["g"] += 1
            sA, sBk = SA_B[2 * g], SA_B[2 * g + 1]
            for qi, (c, qt) in enumerate(it["grp"]):
                i0 = HALO // d + 128 * qt
                rq = c + d * i0 - HALO
                rhs = qTd[hb_][0:64, rq:rq + 127 * d + 1:d] if d > 1 else qTd[hb_][0:64, rq:rq + 128]
                for (bkk, ioff) in ((sA, i0 - 64), (sBk, i0 + 64)):
                    rk = c + d * ioff
                    lhs = kTd[hb_][0:64, rk:rk + 127 * d + 1:d] if d > 1 else kTd[hb_][0:64, rk:rk + 128]
                    P.mm(banks[bkk][:, qi * 128:(qi + 1) * 128], lhs, rhs, True, True, (R_qk[hb_],), (RB[bkk],))
            pts = []
            for ab, bkk in enumerate((sA, sBk)):
                eb = 2 * g + ab
                P.act(ebf[eb], banks[bkk], AF.Exp, (RB[bkk],), (R_ebf[eb],), scale=0.125)
                mk = dmask[:, (h * 3 + di) * 2 + ab, :].unsqueeze(1).broadcast_to([128, 4, 128])
                P.tt("dve", pTd[eb].rearrange("p (q f) -> p q f", f=128),
                     ebf[eb].rearrange("p (q f) -> p q f", f=128), mk, ALU.mult, (R_ebf[eb], R_dm), (R_pTd[eb],))
                pts.append(eb)
            return dict(hb=hb_, vb=vb, pts=pts, boff=state["boff"])

        def stageB(it, st):
            s = it["s"]
            n = NOWN[s]
            d = it["d"]
            di = it["di"]
            hh = it["hh"]
            h = it["hp"] * 2 + hh
            hb_, vb, pts, boff = st["hb"], st["vb"], st["pts"], st["boff"]
            nb = n // (128 * d) + 1
            acc = accd[hb_]
            ob = OD_B[c4["o"] % 2]
            c4["o"] += 1
            grp = it["grp"]
            for qi, (c, qt) in enumerate(grp):
                blkA = boff[di] + c * nb + qt
                for ab in range(2):
                    P.mm(banks[ob][0:65, qi * 128:(qi + 1) * 128], Vd[vb][:, blkA + ab, hh * 65:(hh + 1) * 65],
                         pTd[pts[ab]][:, qi * 128:(qi + 1) * 128], ab == 0, ab == 1,
                         (R_Vd[vb], R_pTd[pts[ab]]), (RB[ob],))
            c_0, qt0 = grp[0]
            if d == 1:
                av = acc[0:65, qt0 * 128:qt0 * 128 + 512]
                pv = banks[ob][0:65, :]
            else:
                base = 128 * d * qt0
                av = acc[0:65, base:base + 128 * d].rearrange("p (f c) -> p c f", c=d)[:, c_0:c_0 + 4, :]
                pv = banks[ob][0:65, :].rearrange("p (c f) -> p c f", f=128)
            if di == 0:
                P.copy("dve", av, pv, (RB[ob],), (R_acc[hb_],))
            else:
                P.tt("dve", av, av, pv, ALU.add, (RB[ob], R_acc[hb_]), (R_acc[hb_],))
            if it["last_head"]:
                for qt in range(n // 512):
                    yb_ = c4["y"] % 2
                    c4["y"] += 1
                    cs_ = slice(qt * 512, (qt + 1) * 512)
                    P.mm(banks[6][0:64, :], ones32[64:65, 0:64], acc[64:65, cs_], True, True, (R_acc[hb_], R_const), (RB[6],))
                    P.act(lnb[0:64, :], banks[6][0:64, :], AF.Ln, (RB[6],), (R_fin4,))
                    P.act(rb4[0:64, :], lnb[0:64, :], AF.Exp, (R_fin4,), (R_fin4,), scale=-1.0)
                    P.tt("dve", ybf4[yb_][0:64, :], acc[0:64, cs_], rb4[0:64, :], ALU.mult, (R_acc[hb_], R_fin4), (R_ybf4[yb_],))
                    P.dma("pool", yb_d[s][h * 64:(h + 1) * 64, qt * 512:(qt + 1) * 512], ybf4[yb_][0:64, :],
                          (R_ybf4[yb_],), (R_yb[s],))

        sts = {0: stageA(items[0])}
        for i in range(len(items)):
            if i + 1 < len(items):
                sts[i + 1] = stageA(items[i + 1])
            stageB(items[i], sts.pop(i))
        P.barrier()
        A.release(m4)

        m5 = A.mark()
        TB = 1024
        R_w5 = Res()
        wob = A.bf(8 * 1024).rearrange("p (c n) -> p c n", n=1024)
        wr32 = A.f32(8 * 20).rearrange("p (j c) -> p j c", c=20)
        selb = A.bf(NEXP * 128, 16)
        NSTG = 4
        wstg = [A.f32(4 * 512) for _ in range(NSTG)]
        R_wstg = [Res() for _ in range(NSTG)]
        sc5 = {"stg": 0}

        def load_cast(dst_bf, src_ap, shape_c, eng):
            k_ = sc5["stg"] % NSTG
            sc5["stg"] += 1
            jj, cc = shape_c
            st = wstg[k_][:, 0:jj * cc].rearrange("p (j c) -> p j c", c=cc)
            P.dma("sp", st, src_ap, (), (R_wstg[k_],))
            return k_, st

        for half in range(4):
            k_, st = load_cast(None, w_out.rearrange("(c p) n -> p c n", p=128)[:, half * 2:(half + 1) * 2, :], (2, 1024), "dve")
            P.copy("dve", wob[:, half * 2:(half + 1) * 2, :], st, (R_wstg[k_],), (R_w5,))
        P.dma("sp", wr32, w_r.rearrange("(j p) c -> p j c", p=128), (), (R_w5,))
        k_ = sc5["stg"] % NSTG
        sc5["stg"] += 1
        P.dma("sp", wstg[k_][0:16, 0:NEXP * 128], sel_in, (), (R_wstg[k_],))
        P.copy("dve", selb, wstg[k_][0:16, 0:NEXP * 128], (R_wstg[k_],), (R_w5,))

        acc5 = A.f32(8 * TB).rearrange("p (j c) -> p j c", c=TB)
        R_acc5 = Res()
        h2b = A.bf(8 * TB).rearrange("p (j c) -> p j c", c=TB)
        R_h2b = Res()
        h2f = A.f32(8 * 512).rearrange("p (j c) -> p j c", c=512)
        R_h2f = Res()
        yab = A.bf(8 * 512).rearrange("p (c n) -> p c n", n=512)
        R_yab = Res()
        sq5 = yab
        R_sq5 = R_yab
        rstd5 = A.f32(512)
        lnt5 = A.f32(512)
        R_rstd5 = Res()
        wcT = A.bf(TB, 16)
        R_wcT = Res()
        wg = [A.bf(8 * 512).rearrange("p (j c) -> p j c", c=512) for _ in range(2)]
        wu = [A.bf(8 * 512).rearrange("p (j c) -> p j c", c=512) for _ in range(2)]
        wd = [A.bf(4 * 1024).rearrange("p (j c) -> p j c", c=1024) for _ in range(2)]
        R_wg = [Res(), Res()]
        R_wu = [Res(), Res()]
        R_wd = [Res(), Res()]
        wcb = [A.bf(512) for _ in range(2)]
        R_wcb = [Res(), Res()]
        sa = [A.bf(512) for _ in range(2)]
        R_sa = [Res(), Res()]
        uw = [A.bf(512) for _ in range(2)]
        R_uw = [Res(), Res()]
        hid = [A.bf(4 * 512).rearrange("p (j c) -> p j c", c=512) for _ in range(2)]
        R_hid = [Res(), Res()]
        rl = A.f32(80).rearrange("p (q c) -> p q c", c=20)
        r_a = A.f32(16).rearrange("p (q c) -> p q c", c=4)
        goh = A.f32(16).rearrange("p (q c) -> p q c", c=4)
        r_m = A.f32(4)
        r_s = A.f32(4)
        gw = A.f32(4)
        esel = A.f32(64).rearrange("p (q g e) -> p q g e", g=4, e=4)
        el = A.f32(16).rearrange("p (q c) -> p q c", c=4)
        ee = A.f32(16).rearrange("p (q c) -> p q c", c=4)
        mk1 = A.f32(16).rearrange("p (q c) -> p q c", c=4)
        ee2 = A.f32(16).rearrange("p (q c) -> p q c", c=4)
        mk2 = A.f32(16).rearrange("p (q c) -> p q c", c=4)
        m1_ = A.f32(4)
        m2_ = A.f32(4)
        wexp = A.f32(16).rearrange("p (q c) -> p q c", c=4)
        wc = A.f32(64).rearrange("p (q g e) -> p q g e", g=4, e=4)
        R_rt = Res()
        yout = [A.f32(512) for _ in range(2)]
        R_yout = [Res(), Res()]

        def bc4(ap):
            return ap.unsqueeze(2).broadcast_to([128, 4, 4])

        c5 = {"pb": 0, "e": 0, "ob": 0, "y": 0, "w": 0}
        PB5 = [0, 1, 2, 3]
        OB5 = [4, 5]
        blocks = []
        for s in "ps":
            for bi in range(NOWN[s] // TB):
                blocks.append((s, bi))
        PIECES = []
        for gb in range(len(blocks)):
            for e in range(NEXP):
                wb_ = (gb * NEXP + e) % 2
                for hf in range(2):
                    PIECES.append((w_gate[e].rearrange("(j p) c -> p j c", p=128)[:, hf * 4:(hf + 1) * 4, :],
                                   wg[wb_][:, hf * 4:(hf + 1) * 4, :], R_wg[wb_], 512))
                for hf in range(2):
                    PIECES.append((w_up[e].rearrange("(j p) c -> p j c", p=128)[:, hf * 4:(hf + 1) * 4, :],
                                   wu[wb_][:, hf * 4:(hf + 1) * 4, :], R_wu[wb_], 512))
                for hf in range(2):
                    PIECES.append((w_down[e].rearrange("(j p) c -> p j c", p=128)[:, hf * 2:(hf + 1) * 2, :],
                                   wd[wb_][:, hf * 2:(hf + 1) * 2, :], R_wd[wb_], 1024))
        pc = {"dma": 0, "cast": 0}
        pstg = {}

        def piece_dma():
            i = pc["dma"]
            if i >= len(PIECES):
                return
            pc["dma"] += 1
            src, dst, Rd, cc = PIECES[i]
            k_ = sc5["stg"] % NSTG
            sc5["stg"] += 1
            st = wstg[k_].rearrange("p (j c) -> p j c", c=cc)
            P.dma("sp", st, src, (), (R_wstg[k_],))
            pstg[i] = (k_, st)

        def piece_cast():
            i = pc["cast"]
            if i >= len(PIECES):
                return
            pc["cast"] += 1
            src, dst, Rd, cc = PIECES[i]
            k_, st = pstg.pop(i)
            P.copy("act", dst, st, (R_wstg[k_],), (Rd,))

        def slot():
            piece_cast()
            piece_dma()

        for _ in range(3):
            piece_dma()
        for _ in range(6):
            slot()
        ucnt = {"u": 0}
        for gb, (s, bi) in enumerate(blocks):
            si = SIDX[s]
            for tl in range(TB // 512):
                q0 = bi * TB + tl * 512
                tc0 = tl * 512
                P.dma("sp", acc5[:, :, tc0:tc0 + 512],
                      xT[s].rearrange("(j p) c -> p j c", p=128)[:, :, HALO + q0:HALO + q0 + 512], (), (R_acc5,))
                P.dma("sp", yab[:, 0:4, :], ya_d[s].rearrange("(c p) n -> p c n", p=128)[:, :, q0:q0 + 512], (R_ya[s],), (R_yab,))
                P.dma("sp", yab[:, 4:8, :], yb_d[s].rearrange("(c p) n -> p c n", p=128)[:, :, q0:q0 + 512], (R_yb[s],), (R_yab,))
                for j in range(8):
                    bk = PB5[c5["pb"] % 4]
                    c5["pb"] += 1
                    for c in range(8):
                        P.mm(banks[bk], wob[:, c, j * 128:(j + 1) * 128], yab[:, c, :], c == 0, c == 7, (R_w5, R_yab), (RB[bk],))
                    P.stt("dve", acc5[:, j, tc0:tc0 + 512], banks[bk], mod[:, 16 + j, si:si + 1], acc5[:, j, tc0:tc0 + 512],
                          ALU.mult, ALU.add, (RB[bk], R_acc5, R_mod), (R_acc5,))
                P.act(sq5, acc5[:, :, tc0:tc0 + 512], AF.Square, (R_acc5,), (R_sq5,))
                for j in range(8):
                    P.mm(banks[6], ones_bf, sq5[:, j, :], j == 0, j == 7, (R_sq5, R_const), (RB[6],))
                rstd_from_ss(rstd5, banks[6], 1.0 / D, (RB[6], R_const), (R_rstd5,), lnt5)
                for j in range(8):
                    P.stt("dve", h2f[:, j, :], acc5[:, j, tc0:tc0 + 512], g2s[:, j, si:si + 1], rstd5, ALU.mult, ALU.mult,
                          (R_acc5, R_rstd5, R_mod), (R_h2f,))
                for j in range(8):
                    P.act(h2f[:, j, :], h2f[:, j, :], AF.Identity, (R_h2f, R_mod), (R_h2f,), bias=mod[:, 24 + j, si:si + 1])
                for j in range(8):
                    P.copy("dve", h2b[:, j, tc0:tc0 + 512], h2f[:, j, :], (R_h2f,), (R_h2b,))
                for q in range(4):
                    for j in range(8):
                        P.mm(banks[7][:, q * 20:(q + 1) * 20], h2f[:, j, q * 128:(q + 1) * 128], wr32[:, j, :], j == 0, j == 7,
                             (R_h2f, R_w5), (RB[7],))
                P.copy("dve", rl, banks[7][:, 0:80].rearrange("p (q c) -> p q c", c=20), (RB[7],), (R_rt,))
                RT = (R_rt,)
                gl = rl[:, :, 0:4]
                P.reduce(r_m, gl, ALU.max, RT, RT)
                P.tt("dve", r_a, gl, bc4(r_m), ALU.subtract, RT, RT)
                P.tt("dve", goh, gl, bc4(r_m), ALU.is_equal, RT, RT)
                P.act(r_a, r_a, AF.Exp, RT, RT)
                P.reduce(r_s, r_a, ALU.add, RT, RT)
                P.recip(gw, r_s, RT, RT)
                P.tt("dve", esel, rl[:, :, 4:20].rearrange("p q (g e) -> p q g e", e=4),
                     goh.unsqueeze(3).broadcast_to([128, 4, 4, 4]), ALU.mult, RT, RT)
                P.reduce(el, esel.rearrange("p q g e -> p q e g"), ALU.add, RT, RT)
                P.reduce(r_m, el, ALU.max, RT, RT)
                P.tt("dve", ee, el, bc4(r_m), ALU.subtract, RT, RT)
                P.act(ee, ee, AF.Exp, RT, RT)
                P.reduce(m1_, ee, ALU.max, RT, RT)
                P.tt("dve", mk1, ee, bc4(m1_), ALU.is_equal, RT, RT)
                P.tt("dve", ee2, ee, mk1, ALU.mult, RT, RT)
                P.tt("dve", ee2, ee, ee2, ALU.subtract, RT, RT)
                P.reduce(m2_, ee2, ALU.max, RT, RT)
                P.tt("dve", mk2, ee2, bc4(m2_), ALU.is_equal, RT, RT)
                P.tt("dve", mk1, mk1, mk2, ALU.add, RT, RT)
                P.tt("dve", m1_, m1_, m2_, ALU.add, RT, RT)
                P.recip(m1_, m1_, RT, RT)
                P.tt("dve", m1_, m1_, gw, ALU.mult, RT, RT)
                P.tt("dve", wexp, ee, mk1, ALU.mult, RT, RT)
                P.tt("dve", wexp, wexp, bc4(m1_), ALU.mult, RT, RT)
                P.tt("dve", wc, goh.unsqueeze(3).broadcast_to([128, 4, 4, 4]),
                     wexp.unsqueeze(2).broadcast_to([128, 4, 4, 4]), ALU.mult, RT, RT)
                for q in range(4):
                    P.tr(banks[6][0:16, q * 128:(q + 1) * 128],
                         wc[:, q].rearrange("p g e -> p (g e)"), ident, (R_rt, R_const), (RB[6],))
                P.copy("dve", wcT[0:16, tc0:tc0 + 512], banks[6][0:16, :], (RB[6],), (R_wcT,))
            units = []
            for e in range(NEXP):
                for tl in range(TB // 512):
                    units.append((e, tl, (gb * NEXP + e) % 2))

            def GU(u):
                e, tl, wbuf = u
                tc0 = tl * 512
                cb = ucnt["u"] % 2
                ucnt["u"] += 1
                P.mm(banks[7], selb[0:16, e * 128:(e + 1) * 128], wcT[0:16, tc0:tc0 + 512], True, True,
                     (R_w5, R_wcT), (RB[7],))
                P.copy("act", wcb[cb], banks[7], (RB[7],), (R_wcb[cb],))
                for fc in range(4):
                    ba = PB5[c5["pb"] % 4]
                    bu = PB5[(c5["pb"] + 1) % 4]
                    c5["pb"] += 2
                    for j in range(8):
                        P.mm(banks[ba], wg[wbuf][:, j, fc * 128:(fc + 1) * 128], h2b[:, j, tc0:tc0 + 512], j == 0, j == 7,
                             (R_wg[wbuf], R_h2b), (RB[ba],))
                    for j in range(8):
                        P.mm(banks[bu], wu[wbuf][:, j, fc * 128:(fc + 1) * 128], h2b[:, j, tc0:tc0 + 512], j == 0, j == 7,
                             (R_wu[wbuf], R_h2b), (RB[bu],))
                    sb_ = fc % 2
                    P.act(sa[sb_], banks[ba], AF.Silu, (RB[ba],), (R_sa[sb_],))
                    P.tt("dve", uw[sb_], banks[bu], wcb[cb], ALU.mult, (RB[bu], R_wcb[cb]), (R_uw[sb_],))
                    P.tt("dve", hid[cb][:, fc, :], sa[sb_], uw[sb_], ALU.mult, (R_sa[sb_], R_uw[sb_]), (R_hid[cb],))
                    if fc < 3:
                        slot()
                return cb

            def DN(u, cb):
                e, tl, wbuf = u
                tc0 = tl * 512
                for j in range(8):
                    ob = OB5[c5["ob"] % 2]
                    c5["ob"] += 1
                    for fc in range(4):
                        P.mm(banks[ob], wd[wbuf][:, fc, j * 128:(j + 1) * 128], hid[cb][:, fc, :], fc == 0, fc == 3,
                             (R_wd[wbuf], R_hid[cb]), (RB[ob],))
                    P.stt("dve", acc5[:, j, tc0:tc0 + 512], banks[ob], mod[:, 40 + j, si:si + 1], acc5[:, j, tc0:tc0 + 512],
                          ALU.mult, ALU.add, (RB[ob], R_acc5, R_mod), (R_acc5,))

            cbs = {0: GU(units[0])}
            for i in range(len(units)):
                if i + 1 < len(units):
                    cbs[i + 1] = GU(units[i + 1])
                DN(units[i], cbs.pop(i))
            for tl in range(TB // 512):
                tc0 = tl * 512
                q0 = bi * TB + tc0
                P.act(sq5, acc5[:, :, tc0:tc0 + 512], AF.Square, (R_acc5,), (R_sq5,))
                for j in range(8):
                    P.mm(banks[6], ones_bf, sq5[:, j, :], j == 0, j == 7, (R_sq5, R_const), (RB[6],))
                rstd_from_ss(rstd5, banks[6], 1.0 / D, (RB[6], R_const), (R_rstd5,), lnt5)
                for j in range(8):
                    yb_ = c5["y"] % 2
                    c5["y"] += 1
                    P.stt("dve", yout[yb_], acc5[:, j, tc0:tc0 + 512], g_fin[:, j:j + 1], rstd5, ALU.mult, ALU.mult,
                          (R_acc5, R_rstd5, R_vec), (R_yout[yb_],))
                    P.dma("pool", yT[s][j * 128:(j + 1) * 128, q0:q0 + 512], yout[yb_], (R_yout[yb_],), ())
        A.release(m5)

        with nc.Block() as block:
            P.emit(block, csem, dsem)
    return nc


def _rope_tables(S):
    inv = (np.float32(10000.0) ** (-np.arange(0, 32, 2, dtype=np.float32) / np.float32(32))).astype(np.float32)
    ang = (np.arange(S, dtype=np.float32)[:, None] * inv[None, :]).astype(np.float32)
    cos = np.cos(ang).astype(np.float32).T
    sin = np.sin(ang).astype(np.float32).T
    cs = np.concatenate([cos, cos], axis=0)
    sn = np.concatenate([-sin, sin], axis=0)
    return cs, sn


def _dmask():
    slopes = (2.0 ** (-8.0 * np.arange(1, 9) / 8)).astype(np.float32)
    p = np.arange(128)[:, None]
    f = np.arange(128)[None, :]
    out = np.zeros((128, 48, 128), np.float32)
    for h in range(8):
        for di, d in enumerate(DPAT):
            dA = p - 64 - f
            mA = np.where(p >= f, np.exp(-slopes[h] * np.abs(dA).astype(np.float32) * d), 0.0)
            dB = p + 64 - f
            mB = np.where(p <= f, np.exp(-slopes[h] * np.abs(dB).astype(np.float32) * d), 0.0)
            out[:, (h * 3 + di) * 2 + 0, :] = mA
            out[:, (h * 3 + di) * 2 + 1, :] = mB
    return out.reshape(128, 48 * 128)


_NC_CACHE = {}


def kernel(x_prompt, x_sample, c_prompt, c_sample, ada_w, ada_b, norm_mix_g, w_in, q_norm_g, kv_norm_g,
           w_uq, w_ukv, w_out, norm_moe_g, w_router_group, w_router_expert, w_gate, w_up, w_down,
           final_norm_g, _debug_out=None):
    f = np.float32
    x_prompt = np.asarray(x_prompt, f)
    x_sample = np.asarray(x_sample, f)
    w_in0 = np.ascontiguousarray(np.asarray(w_in, f)[0])
    w_uq0 = np.asarray(w_uq, f)[0]
    pe = w_in0[:, 384:416]
    w_pe_sw = np.ascontiguousarray(np.concatenate([pe[:, 16:32], pe[:, 0:16]], axis=1))
    wq = w_uq0.reshape(256, 8, 96)
    wq_sw = wq.copy()
    wq_sw[:, :, 64:80] = wq[:, :, 80:96]
    wq_sw[:, :, 80:96] = wq[:, :, 64:80]
    w_uq_sw = np.ascontiguousarray(wq_sw.reshape(256, 768))
    w_r = np.ascontiguousarray(np.concatenate([np.asarray(w_router_group, f)[0],
                                               np.asarray(w_router_expert, f)[0].reshape(D, 16)], axis=1))
    cs_full = {"p": _rope_tables(SP), "s": _rope_tables(SS)}
    dmask = _dmask()
    ident = np.eye(128, dtype=f)
    sel = np.zeros((16, NEXP * 128), f)
    for e in range(NEXP):
        sel[e, e * 128:(e + 1) * 128] = 1.0
    shared = {
        "ada_w": np.ascontiguousarray(np.asarray(ada_w, f)[0]),
        "w_in": w_in0, "w_pe_sw": w_pe_sw,
        "w_uq": np.ascontiguousarray(w_uq0), "w_uq_sw": w_uq_sw,
        "w_ukv": np.ascontiguousarray(np.asarray(w_ukv, f)[0]),
        "w_out": np.ascontiguousarray(np.asarray(w_out, f)[0]),
        "w_r": w_r,
        "w_gate": np.ascontiguousarray(np.asarray(w_gate, f)[0]),
        "w_up": np.ascontiguousarray(np.asarray(w_up, f)[0]),
        "w_down": np.ascontiguousarray(np.asarray(w_down, f)[0]),
        "dmask": dmask, "ident": ident, "sel": sel,
    }

    def chunked(v, nch):
        return np.asarray(v, f).reshape(nch, 128).T

    xT_full = {}
    for g in range(2):
        xT_full[("p", g)] = np.ascontiguousarray(x_prompt[g].T)
        xT_full[("s", g)] = np.ascontiguousarray(x_sample[g].T)
    in_maps = []
    for core in range(8):
        g, j = core // 4, core % 4
        m = dict(shared)
        c2 = np.stack([np.asarray(c_prompt, f)[g], np.asarray(c_sample, f)[g]], axis=1)
        vecs = np.zeros((128, 96), f)
        vecs[:, 0:16] = c2.reshape(8, 128, 2).transpose(1, 0, 2).reshape(128, 16)
        vecs[:, 16:64] = chunked(np.asarray(ada_b, f)[0], 48)
        vecs[:, 64:72] = chunked(np.asarray(norm_mix_g, f)[0], 8)
        vecs[:, 72:80] = chunked(np.asarray(norm_moe_g, f)[0], 8)
        vecs[:, 80:88] = chunked(np.asarray(final_norm_g, f), 8)
        vecs[:, 88:90] = chunked(np.asarray(q_norm_g, f)[0], 2)
        vecs[:, 90:91] = chunked(np.asarray(kv_norm_g, f)[0], 1)
        m["vecs"] = vecs
        for s, S_, n in (("p", SP, NP_), ("s", SS, NS_)):
            o = j * n
            idx = (o - HALO + np.arange(S_)) % S_
            m["xT" + s] = np.ascontiguousarray(xT_full[(s, g)][:, idx])
            cs, sn = cs_full[s]
            m["cs_" + s] = np.ascontiguousarray(cs[:, idx])
            m["sn_" + s] = np.ascontiguousarray(sn[:, idx])
            u = o - HALO + np.arange(n + 2 * HALO)
            val = ((u >= 0) & (u < S_)).astype(f)
            m["valid_" + s] = np.ascontiguousarray(val.reshape(-1, 128).T)
        in_maps.append(m)

    if "nc" not in _NC_CACHE:
        _NC_CACHE["nc"] = build_program()
    nc = _NC_CACHE["nc"]
    res = run_bass_kernel_spmd(nc, in_maps, core_ids=list(range(8)))
    y_prompt = np.empty((2, SP, D), f)
    y_sample = np.empty((2, SS, D), f)
    for core in range(8):
        g, j = core // 4, core % 4
        r = res.results[core]
        y_prompt[g, j * NP_:(j + 1) * NP_, :] = np.asarray(r["yTp"]).T
        y_sample[g, j * NS_:(j + 1) * NS_, :] = np.asarray(r["yTs"]).T
    if _debug_out is not None:
        _debug_out.append(res)
    return (y_prompt, y_sample)


if __name__ == "__main__":
    import time
    t = time.time()
    nc = build_program()
    print("build ok", time.time() - t)
```
